# Optimizing a Trainium2 kernel written in Bass

```python
import math
import jax, jax.numpy as jnp
from jax import lax
import numpy as np

D_MODEL = 1024
BATCH = 4
SEQ = 4096
DEPTH = 1

MLA_HEADS = 8
MLA_Q_LORA = 256
MLA_KV_LORA = 128
MLA_NOPE = 64
MLA_ROPE = 32
MLA_V = 64
MLA_QK = MLA_NOPE + MLA_ROPE
MLA_WIDTH = MLA_HEADS * MLA_V
ROPE_THETA = 10000.0
FOX_HEADS = 8
FOX_HEAD_DIM = 64
FOX_WIDTH = FOX_HEADS * FOX_HEAD_DIM
Q_BLOCK = 128
IN_SPLIT_SIZES = (MLA_Q_LORA, MLA_KV_LORA, MLA_ROPE, FOX_WIDTH, FOX_WIDTH, FOX_WIDTH, FOX_HEADS, D_MODEL, D_MODEL)
IN_COLS = MLA_Q_LORA + MLA_KV_LORA + MLA_ROPE + 3 * FOX_WIDTH + FOX_HEADS + 2 * D_MODEL
N_EXPERTS = 64
N_GROUPS = 8
TOPK_GROUPS = 4
TOP_K = 6
D_EXPERT = 256
D_SHARED = 256
ROUTED_SCALE = 2.5
EXPERT_BLOCK = 128
N_MOD = 6
NORM_EPS = 1e-6
NEG_INF = -1e30

kernel_name = 'adaln_mla_fox_gated_hybrid_moe'


def rms_norm(x, gain):
    xf = x.astype(jnp.float32)
    y = xf * lax.rsqrt(jnp.mean(xf * xf, axis=-1, keepdims=True) + NORM_EPS)
    return (y * gain.astype(jnp.float32)).astype(x.dtype)


def split_cols(t, sizes):
    out, start = [], 0
    for n in sizes:
        out.append(t[..., start:start + n])
        start += n
    return out


def apply_rope(x, positions):
    half = x.shape[-1] // 2
    inv_freq = jnp.power(ROPE_THETA, -jnp.arange(half, dtype=jnp.float32) / half)
    ang = positions.astype(jnp.float32)[:, None, :, None] * inv_freq
    cos = jnp.cos(ang).astype(x.dtype)
    sin = jnp.sin(ang).astype(x.dtype)
    x1, x2 = x[..., :half], x[..., half:]
    return jnp.concatenate([x1 * cos - x2 * sin, x2 * cos + x1 * sin], axis=-1)


def causal_block_attention(q, k, v, scale, decay=None):
    B, H, S, _ = q.shape
    dv = v.shape[-1]
    nblk = S // Q_BLOCK

    def to_blocks(t):
        return jnp.moveaxis(t.reshape((B, H, nblk, Q_BLOCK) + t.shape[3:]), 2, 0)

    key_pos = jnp.arange(S)

    def one_block(xs):
        i, qb = xs[0], xs[1]
        logits = jnp.einsum('bhqd,bhkd->bhqk', qb, k).astype(jnp.float32) * scale
        if decay is not None:
            logits = logits + xs[2][..., :, None] - decay[:, :, None, :]
        q_pos = i * Q_BLOCK + jnp.arange(Q_BLOCK)
        logits = jnp.where(key_pos[None, :] <= q_pos[:, None], logits, NEG_INF)
        p = jax.nn.softmax(logits, axis=-1).astype(v.dtype)
        return jnp.einsum('bhqk,bhkd->bhqd', p, v)

    xs = (jnp.arange(nblk), to_blocks(q))
    if decay is not None:
        xs = xs + (to_blocks(decay),)
    out = lax.map(one_block, xs)
    return jnp.moveaxis(out, 0, 2).reshape(B, H, S, dv)


def hybrid_mixer(h, positions, w_in, b_forget, g_q_lat, w_q_up, g_kv_lat, w_kv_up, w_o_mla, w_o_fox, w_out):
    B, S, _ = h.shape
    proj = h @ w_in
    q_lat, kv_lat, k_rope, fq, fk, fv, f_logit, gate_a, gate_b = split_cols(proj, IN_SPLIT_SIZES)

    q = (rms_norm(q_lat, g_q_lat) @ w_q_up).reshape(B, S, MLA_HEADS, MLA_QK).transpose(0, 2, 1, 3)
    q_nope, q_pe = q[..., :MLA_NOPE], apply_rope(q[..., MLA_NOPE:], positions)
    kv = (rms_norm(kv_lat, g_kv_lat) @ w_kv_up).reshape(B, S, MLA_HEADS, MLA_NOPE + MLA_V).transpose(0, 2, 1, 3)
    k_nope, v_mla = kv[..., :MLA_NOPE], kv[..., MLA_NOPE:]
    k_pe = apply_rope(k_rope[:, None, :, :], positions)
    q_mla = jnp.concatenate([q_nope, q_pe], axis=-1)
    k_mla = jnp.concatenate([k_nope, jnp.broadcast_to(k_pe, (B, MLA_HEADS, S, MLA_ROPE))], axis=-1)
    o_mla = causal_block_attention(q_mla, k_mla, v_mla, 1.0 / math.sqrt(MLA_QK))
    o_mla = o_mla.transpose(0, 2, 1, 3).reshape(B, S, MLA_WIDTH)

    def heads(t):
        return t.reshape(B, S, FOX_HEADS, FOX_HEAD_DIM).transpose(0, 2, 1, 3)

    log_f = jax.nn.log_sigmoid((f_logit + b_forget).astype(jnp.float32))
    cum_log_f = jnp.cumsum(log_f, axis=1).transpose(0, 2, 1)
    o_fox = causal_block_attention(heads(fq), heads(fk), heads(fv), 1.0 / math.sqrt(FOX_HEAD_DIM), cum_log_f)
    o_fox = o_fox.transpose(0, 2, 1, 3).reshape(B, S, FOX_WIDTH)

    merged = jax.nn.sigmoid(gate_a) * (o_mla @ w_o_mla) + jax.nn.sigmoid(gate_b) * (o_fox @ w_o_fox)
    return merged @ w_out


def moe_ffn(h, w_router, b_router, w_exp_gate, w_exp_up, w_exp_down, w_sh_gate, w_sh_up, w_sh_down):
    B, S, D = h.shape
    T = B * S
    xf = h.reshape(T, D)

    scores = jax.nn.sigmoid((xf @ w_router).astype(jnp.float32))
    choice = scores + b_router.astype(jnp.float32)
    group_score = lax.top_k(choice.reshape(T, N_GROUPS, N_EXPERTS // N_GROUPS), 2)[0].sum(-1)
    _, group_idx = lax.top_k(group_score, TOPK_GROUPS)
    group_keep = (group_idx[..., None] == jnp.arange(N_GROUPS)).any(axis=-2)
    expert_keep = jnp.repeat(group_keep, N_EXPERTS // N_GROUPS, axis=-1)
    _, expert_idx = lax.top_k(jnp.where(expert_keep, choice, NEG_INF), TOP_K)
    gate = jnp.take_along_axis(scores, expert_idx, axis=-1)
    gate = (gate / gate.sum(-1, keepdims=True) * ROUTED_SCALE).astype(h.dtype)

    n_assign = T * TOP_K
    flat_e = expert_idx.reshape(-1).astype(jnp.int32)
    flat_tok = jnp.arange(n_assign, dtype=jnp.int32) // TOP_K
    order = jnp.argsort(flat_e)
    se, stok, sgate = flat_e[order], flat_tok[order], gate.reshape(-1)[order]
    counts = jnp.zeros((N_EXPERTS,), jnp.int32).at[flat_e].add(1)
    padded = (counts + EXPERT_BLOCK - 1) // EXPERT_BLOCK * EXPERT_BLOCK
    start = jnp.cumsum(counts) - counts
    pend = jnp.cumsum(padded)
    pstart = pend - padded
    dest = pstart[se] + jnp.arange(n_assign, dtype=jnp.int32) - start[se]
    n_blocks = (n_assign + EXPERT_BLOCK - 1) // EXPERT_BLOCK + N_EXPERTS
    n_rows = n_blocks * EXPERT_BLOCK
    row_tok = jnp.full((n_rows,), T, jnp.int32).at[dest].set(stok)
    row_gate = jnp.zeros((n_rows,), h.dtype).at[dest].set(sgate)
    block_e = jnp.minimum(
        jnp.searchsorted(pend, jnp.arange(n_blocks, dtype=jnp.int32) * EXPERT_BLOCK, side='right'),
        N_EXPERTS - 1)
    x_pad = jnp.concatenate([xf, jnp.zeros((1, D), xf.dtype)], axis=0)

    def expert_block(xs):
        tok, g, e = xs
        xb = x_pad[tok]
        hb = jax.nn.silu(xb @ w_exp_gate[e]) * (xb @ w_exp_up[e])
        return (hb @ w_exp_down[e]) * g[:, None]

    yb = lax.map(expert_block, (row_tok.reshape(n_blocks, EXPERT_BLOCK),
                                row_gate.reshape(n_blocks, EXPERT_BLOCK), block_e))
    routed = jax.ops.segment_sum(yb.reshape(n_rows, D), row_tok, num_segments=T + 1)[:T]
    shared = (jax.nn.silu(xf @ w_sh_gate) * (xf @ w_sh_up)) @ w_sh_down
    return (shared + routed).reshape(B, S, D)


def dense_init(k, shape, fan_in, scale=1.0):
    return scale * fan_in ** -0.5 * jax.random.normal(k, shape, jnp.float32)


def setup_inputs(seed: int = 0) -> dict:
    key = jax.random.key(seed)
    ks = jax.random.split(key, 25)
    L, D, E = DEPTH, D_MODEL, N_EXPERTS

    def gain(k, shape):
        return 1.0 + 0.02 * jax.random.normal(k, shape, jnp.float32)

    offset = jax.random.randint(ks[2], (BATCH, 1), 0, 1024, dtype=jnp.int32)
    return {
        'x': jax.random.normal(ks[0], (BATCH, SEQ, D), jnp.float32),
        'c': jax.random.normal(ks[1], (BATCH, D), jnp.float32),
        'positions': jnp.arange(SEQ, dtype=jnp.int32)[None, :] + offset,
        'w_mod': dense_init(ks[3], (L, D, N_MOD * D), D, 0.5),
        'b_mod': 0.02 * jax.random.normal(ks[4], (L, N_MOD * D), jnp.float32),
        'g_mix_norm': gain(ks[5], (L, D)),
        'w_in': dense_init(ks[6], (L, D, IN_COLS), D),
        'b_forget': 1.0 + 3.0 * jax.random.uniform(ks[7], (L, FOX_HEADS), jnp.float32),
        'g_q_lat': gain(ks[8], (L, MLA_Q_LORA)),
        'w_q_up': dense_init(ks[9], (L, MLA_Q_LORA, MLA_HEADS * MLA_QK), MLA_Q_LORA),
        'g_kv_lat': gain(ks[10], (L, MLA_KV_LORA)),
        'w_kv_up': dense_init(ks[11], (L, MLA_KV_LORA, MLA_HEADS * (MLA_NOPE + MLA_V)), MLA_KV_LORA),
        'w_o_mla': dense_init(ks[12], (L, MLA_WIDTH, D), MLA_WIDTH),
        'w_o_fox': dense_init(ks[13], (L, FOX_WIDTH, D), FOX_WIDTH),
        'w_out': dense_init(ks[14], (L, D, D), D),
        'g_ffn_norm': gain(ks[15], (L, D)),
        'w_router': dense_init(ks[16], (L, D, E), D),
        'b_router': 0.01 * jax.random.normal(ks[17], (L, E), jnp.float32),
        'w_exp_gate': dense_init(ks[18], (L, E, D, D_EXPERT), D),
        'w_exp_up': dense_init(ks[19], (L, E, D, D_EXPERT), D),
        'w_exp_down': dense_init(ks[20], (L, E, D_EXPERT, D), D_EXPERT),
        'w_sh_gate': dense_init(ks[21], (L, D, D_SHARED), D),
        'w_sh_up': dense_init(ks[22], (L, D, D_SHARED), D),
        'w_sh_down': dense_init(ks[23], (L, D_SHARED, D), D_SHARED),
        'g_final': gain(ks[24], (D,)),
    }


def reference(x, c, positions, w_mod, b_mod, g_mix_norm, w_in, b_forget, g_q_lat, w_q_up, g_kv_lat,
              w_kv_up, w_o_mla, w_o_fox, w_out, g_ffn_norm, w_router, b_router, w_exp_gate, w_exp_up,
              w_exp_down, w_sh_gate, w_sh_up, w_sh_down, g_final):
    cond = jax.nn.silu(c)
    for l in range(DEPTH):
        mod = cond @ w_mod[l] + b_mod[l]
        shift_m, scale_m, gate_m, shift_f, scale_f, gate_f = [m[:, None, :] for m in jnp.split(mod, N_MOD, axis=-1)]
        h = rms_norm(x, g_mix_norm[l]) * (1.0 + scale_m) + shift_m
        x = x + gate_m * hybrid_mixer(h, positions, w_in[l], b_forget[l], g_q_lat[l], w_q_up[l], g_kv_lat[l],
                                      w_kv_up[l], w_o_mla[l], w_o_fox[l], w_out[l])
        h = rms_norm(x, g_ffn_norm[l]) * (1.0 + scale_f) + shift_f
        x = x + gate_f * moe_ffn(h, w_router[l], b_router[l], w_exp_gate[l], w_exp_up[l], w_exp_down[l],
                                 w_sh_gate[l], w_sh_up[l], w_sh_down[l])
    return rms_norm(x, g_final)
```

```python
import numpy as np
import ml_dtypes
import concourse.bass as bass
import concourse.mybir as mybir
from concourse.bass_utils import run_bass_kernel_spmd

F32 = mybir.dt.float32
BF16 = mybir.dt.bfloat16
I32 = mybir.dt.int32
U32 = mybir.dt.uint32
AF = mybir.ActivationFunctionType
ALU = mybir.AluOpType
AX = mybir.AxisListType

ENGS = ("pe", "act", "dve", "pool", "sp")
N_DSEM = 32
MAX_POOL_DMAS = 14


class Op:
    __slots__ = ("eng", "fn", "deps", "signal", "sig_idx", "is_dma", "dsem", "dval", "idx")

    def __init__(self, eng, fn, is_dma):
        self.eng = eng
        self.fn = fn
        self.deps = set()
        self.signal = False
        self.sig_idx = 0
        self.is_dma = is_dma
        self.dsem = -1
        self.dval = 0
        self.idx = -1


class Sched:
    def __init__(self, nc):
        self.nc = nc
        self.ops = []
        self.last_w = {}
        self.readers = {}
        self.fence_ops = None
        self.fenced = set()
        self.last_on = {e: None for e in ENGS}
        self.dma_since_fence = []
        self.n_dma = 0
        self.dsem_last = [None] * N_DSEM
        self.pool_dmas = []

    def op(self, eng, fn, reads=(), writes=(), dma=False):
        o = Op(eng, fn, dma)
        o.idx = len(self.ops)
        self.ops.append(o)
        if self.fence_ops is not None and eng not in self.fenced:
            for d in self.fence_ops:
                o.deps.add(d)
            self.fenced.add(eng)
        for k in reads:
            w = self.last_w.get(k)
            if w is not None:
                o.deps.add(w)
        for k in writes:
            w = self.last_w.get(k)
            if w is not None and (dma or self.ops[w].is_dma or self.ops[w].eng != eng):
                o.deps.add(w)
            for r in self.readers.get(k, ()):
                if dma or self.ops[r].is_dma or self.ops[r].eng != eng:
                    o.deps.add(r)
        for k in reads:
            self.readers.setdefault(k, []).append(o.idx)
        for k in writes:
            self.last_w[k] = o.idx
            self.readers[k] = []
        if dma and eng == "pool":
            self.pool_dmas.append(o.idx)
            if len(self.pool_dmas) > MAX_POOL_DMAS:
                o.deps.add(self.pool_dmas[-1 - MAX_POOL_DMAS])
        if dma:
            k = self.n_dma % N_DSEM
            self.n_dma += 1
            prev = self.dsem_last[k]
            if prev is not None:
                o.deps.add(prev)
            o.dsem = k
            o.dval = (self.ops[prev].dval if prev is not None else 0) + 16
            self.dsem_last[k] = o.idx
            self.dma_since_fence.append(o.idx)
        o.deps.discard(o.idx)
        self.last_on[eng] = o.idx
        return o.idx

    def fence(self):
        deps = [v for v in self.last_on.values() if v is not None]
        deps += self.dma_since_fence
        self.fence_ops = list(set(deps))
        self.fenced = set()
        self.dma_since_fence = []

    def emit(self, sems, dsems, final_waits=True):
        nc = self.nc
        ops = self.ops
        for o in ops:
            nd = set()
            for d in o.deps:
                do = ops[d]
                if do.is_dma:
                    nd.add(d)
                elif do.eng == o.eng and do.eng in ("pe", "sp"):
                    continue
                else:
                    do.signal = True
                    nd.add(d)
            o.deps = nd
        cnt = {e: 0 for e in ENGS}
        for o in ops:
            if not o.is_dma and o.signal:
                cnt[o.eng] += 1
                o.sig_idx = cnt[o.eng]
        per_eng = {e: [] for e in ENGS}
        for o in ops:
            per_eng[o.eng].append(o)
        engobj = {"pe": nc.tensor, "act": nc.scalar, "dve": nc.vector, "pool": nc.gpsimd, "sp": nc.sync}
        tail = [o.idx for o in ops if o.is_dma][-N_DSEM:]

        def run(ename, eobj):
            seen = {}
            for o in per_eng[ename]:
                for d in sorted(o.deps):
                    do = ops[d]
                    if do.is_dma:
                        key, val, sem = ("d", do.dsem), do.dval, dsems[do.dsem]
                    else:
                        key, val, sem = ("c", do.eng), do.sig_idx, sems[do.eng]
                    if seen.get(key, 0) >= val:
                        continue
                    seen[key] = val
                    eobj.wait_ge(sem, val)
                ins = o.fn(eobj)
                if o.is_dma:
                    ins.then_inc(dsems[o.dsem], 16)
                elif o.signal:
                    ins.then_inc(sems[o.eng], 1)
            if ename == "sp" and final_waits:
                for d in tail:
                    do = ops[d]
                    eobj.wait_ge(dsems[do.dsem], do.dval)

        with nc.Block() as block:
            @block.tensor
            def _(e):
                run("pe", e)

            @block.scalar
            def _(e):
                run("act", e)

            @block.vector
            def _(e):
                run("dve", e)

            @block.gpsimd
            def _(e):
                run("pool", e)

            @block.sync
            def _(e):
                run("sp", e)


import math

D = 1024
SEQ = 4096
NT_ALL = 32
NT_OWN = 16
EPS = 1e-6
PI = math.pi
SC_MLA = 1.0 / math.sqrt(96.0)
SC_FOX = 1.0 / 8.0
N_EXP = 64
BLK = 128
NBLK = (2048 * 6) // BLK + N_EXP
NSLOT = NBLK * BLK


def _dsize(dt):
    return mybir.dt.size(dt)


class Mem:
    def __init__(self, nc, base=16512, limit=229344):
        self.nc = nc
        self.top = base
        self.limit = limit
        self.n = 0
        self.peak = base

    def alloc(self, name, shape, dt):
        sz = int(np.prod(shape[1:])) * _dsize(dt)
        sz = (sz + 63) // 64 * 64
        t = self.nc.alloc_sbuf_tensor_at("%s_%d" % (name, self.n), list(shape), dt, offset=self.top)
        self.n += 1
        self.top += sz
        self.peak = max(self.peak, self.top)
        assert self.top <= self.limit, ("SBUF overflow", name, self.top)
        return t

    def mark(self):
        return self.top

    def release(self, m):
        self.top = m


class Prog:
    def __init__(self, stage=99, debug=False):
        self.stage = stage
        self.debug = debug
        nc = bass.Bass("TRN2", target_bir_lowering=False)
        self.nc = nc
        self.S = Sched(nc)
        self.M = Mem(nc)
        self.I = {}
        self.uid = 0
        self.ps = [nc.alloc_psum_tensor("psb%d" % i, [128, 512], F32) for i in range(8)]
        self.psb = [p[:].bitcast(BF16) for p in self.ps]

    def din(self, name, shape, dt=F32):
        ap = self.nc.dram_tensor(name, list(shape), dt, kind="ExternalInput").ap()
        self.I[name] = ap
        return ap

    def key(self, base):
        self.uid += 1
        return (base, self.uid)

    def bc_reg(self, e, val):
        if not hasattr(self, "_bc"):
            self._bc = {}
        if val not in self._bc:
            self._bc[val] = e.to_reg(val)
        return self._bc[val]

    def dma(self, out, in_, r=(), w=(), eng="sp"):
        return self.S.op(eng, lambda e: e.dma_start(out=out, in_=in_), r, w, dma=True)

    def mm(self, out, lhsT, rhs, start=True, stop=True, r=(), w=(), sgc=False):
        return self.S.op("pe", lambda e: e.matmul(out, lhsT=lhsT, rhs=rhs, start=start, stop=stop, skip_group_check=sgc), r, w)

    def tr(self, out, in_, ident, r=(), w=()):
        return self.S.op("pe", lambda e: e.transpose(out, in_, ident), r, w)

    def act(self, out, in_, func, r=(), w=(), **kw):
        return self.S.op("act", lambda e: e.activation(out=out, in_=in_, func=func, **kw), r, w)

    def ts(self, eng, out, in0, s1, s2, op0, op1=None, r=(), w=()):
        if op1 is None:
            return self.S.op(eng, lambda e: e.tensor_scalar(out=out, in0=in0, scalar1=s1, scalar2=None, op0=op0), r, w)
        return self.S.op(eng, lambda e: e.tensor_scalar(out=out, in0=in0, scalar1=s1, scalar2=s2, op0=op0, op1=op1), r, w)

    def tt(self, eng, out, in0, in1, op, r=(), w=()):
        return self.S.op(eng, lambda e: e.tensor_tensor(out=out, in0=in0, in1=in1, op=op), r, w)

    def stt(self, eng, out, in0, scalar, in1, op0, op1, r=(), w=(), accum_out=None):
        if accum_out is None:
            return self.S.op(eng, lambda e: e.scalar_tensor_tensor(out=out, in0=in0, scalar=scalar, in1=in1, op0=op0, op1=op1), r, w)
        return self.S.op(eng, lambda e: e.scalar_tensor_tensor(out=out, in0=in0, scalar=scalar, in1=in1, op0=op0, op1=op1, accum_out=accum_out), r, w)

    def copy(self, eng, out, in_, r=(), w=()):
        if eng == "act":
            return self.S.op("act", lambda e: e.copy(out=out, in_=in_), r, w)
        return self.S.op(eng, lambda e: e.tensor_copy(out=out, in_=in_), r, w)

    def memset(self, eng, ap, val, w=()):
        return self.S.op(eng, lambda e: e.memset(ap, val), (), w)

    def tss(self, eng, out, in_, scalar, op, r=(), w=()):
        return self.S.op(eng, lambda e: e.tensor_single_scalar(out=out, in_=in_, scalar=scalar, op=op), r, w)

    def declare_inputs(self):
        d = self.din
        d("x_all", [SEQ, D]); d("x_own", [2048, D])
        d("pos_all", [SEQ], I32); d("pos_own", [2048], I32)
        d("c_t", [128, 8])
        d("w_mod", [D, 6 * D]); d("bmod_fm", [128, 32]); d("bmod_g", [2, D])
        d("g_mix_fm", [128, 8]); d("g_ffn_fm", [128, 8]); d("g_final", [D])
        d("w_in", [D, 4008]); d("bf72", [128, 1]); d("gq_fm", [128, 2]); d("gkv_fm", [128, 1])
        d("w_q_up", [256, 768]); d("w_kv_up", [128, 1024])
        d("w_o_mla", [512, D]); d("w_o_fox", [512, D]); d("w_out", [D, D])
        d("w_router", [D, 64]); d("b_router", [64])
        d("w_exp_gate", [64, D, 256]); d("w_exp_up", [64, D, 256]); d("w_exp_down", [64, 256, D])
        d("w_sh_gate", [D, 256]); d("w_sh_up", [D, 256]); d("w_sh_down", [256, D])
        d("ident", [128, 128]); d("invf", [128, 1]); d("pvec", [128, 2])
        d("mask0", [128, 128]); d("mask1", [128, 128]); d("esel", [128, 1024])
        d("sel3", [128, 24]); d("thr16", [128, 16]); d("thrj", [128, NBLK]); d("iota_p", [128, 1]); d("triu", [128, 128])
        nc = self.nc
        self.scr_kf = nc.dram_tensor("scr_kf", [128, 4, SEQ], BF16, kind="Internal").ap()
        self.scr_vf = nc.dram_tensor("scr_vf", [128, 32, 520], BF16, kind="Internal").ap()
        self.scr_x2 = nc.dram_tensor("scr_x2", [2048, D], F32, kind="Internal").ap()
        self.wall_s = nc.dram_tensor("wall_s", [N_EXP * 128, 6144], BF16, kind="Internal").ap()
        self.xs = nc.dram_tensor("xs", [NSLOT, D], BF16, kind="Internal").ap()
        self.ys = nc.dram_tensor("ys", [NSLOT, D], BF16, kind="Internal").ap()
        self.out = nc.dram_tensor("out", [2048, D], F32, kind="ExternalOutput").ap()
        self.dbg = {}

    def dbg_out(self, name, shape, dt):
        ap = self.nc.dram_tensor(name, list(shape), dt, kind="ExternalOutput").ap()
        self.dbg[name] = ap
        return ap

    def setup_persistent(self):
        M, I = self.M, self.I
        a = M.alloc
        self.ident_f = a("ident_f", [128, 128], F32)
        self.ident_b = a("ident_b", [128, 128], BF16)
        self.m0 = a("m0", [128, 128], BF16)
        self.m1 = a("m1", [128, 128], BF16)
        self.esel = a("esel", [128, 8, 128], BF16)
        self.invf = a("invf", [128, 1], F32)
        self.pvec = a("pvec", [128, 2], F32)
        self.negbf = a("negbf", [128, 1], F32)
        self.mhalf = a("mhalf", [128, 1], F32)
        self.cond = a("cond", [128, 8], F32)
        self.modfm = a("modfm", [128, 32], F32)
        self.am = a("am", [128, 8], F32)
        self.af = a("af", [128, 8], F32)
        self.gkv = a("gkv", [128, 1], F32)
        self.gq = a("gq", [128, 2], F32)
        self.stat = a("stat", [128, 3, 256], F32)
        self.fkT = a("fkT", [128, 32, 8], F32)
        self.fkT2 = a("fkT2", [128, 32, 8], F32)
        self.pneg = a("pneg", [128, 1], F32)
        self.f3 = a("f3", [128, 2048], BF16)
        self.scn = 0
        self.dma(self.ident_f[:], I["ident"], w=["ident_f"])
        self.dma(self.ident_b[:], I["ident"], w=["ident_b"], eng="pool")
        self.dma(self.m0[:], I["mask0"], w=["m0"], eng="pool")
        self.dma(self.m1[:], I["mask1"], w=["m1"], eng="pool")
        self.dma(self.esel[:].rearrange("p h c -> p (h c)"), I["esel"], w=["esel"], eng="pool")
        self.sel3 = a("sel3", [128, 8, 3], BF16)
        self.dma(self.sel3[:].rearrange("p h c -> p (h c)"), I["sel3"], w=["sel3"], eng="pool")
        self.dma(self.invf[:], I["invf"], w=["invf"])
        self.dma(self.pvec[:], I["pvec"], w=["pvec"])
        self.dma(self.negbf[:], I["bf72"], w=["negbf0"])
        self.ts("dve", self.negbf[:], self.negbf[:], -1.0, None, ALU.mult, r=["negbf0"], w=["negbf"])
        self.memset("dve", self.mhalf[:], -0.5, w=["mhalf"])
        self.dma(self.gkv[:], I["gkv_fm"], w=["gkv"])
        self.dma(self.gq[:], I["gq_fm"], w=["gq"])
        self.memset("dve", self.f3[:], 0.0, w=["f3"])

    def stat_col(self):
        c = self.scn
        self.scn += 1
        assert c < 256
        return c

    def phase_a(self):
        M, I, ps = self.M, self.I, self.ps
        mk = M.mark()
        ct = M.alloc("ct", [128, 8], F32)
        bmod = M.alloc("bmod", [128, 32], F32)
        gmix = M.alloc("gmix", [128, 8], F32)
        gffn = M.alloc("gffn", [128, 8], F32)
        tmp8 = M.alloc("tmp8", [128, 8], F32)
        wch = [M.alloc("wch%d" % i, [128, 8, 512], BF16) for i in range(3)]
        condb = M.alloc("condb", [128, 8], BF16)
        self.dma(ct[:], I["c_t"], w=["ct"])
        self.dma(bmod[:], I["bmod_fm"], w=["bmod"])
        self.dma(gmix[:], I["g_mix_fm"], w=["gmix"])
        self.dma(gffn[:], I["g_ffn_fm"], w=["gffn"])
        self.act(self.cond[:], ct[:], AF.Silu, r=["ct"], w=["cond"])
        self.copy("dve", condb[:], self.cond[:], r=["cond"], w=["condb"])
        wm = I["w_mod"].rearrange("(kc p) n -> p kc n", p=128)
        secs = [0, 1, 3, 4]
        ci = 0
        for si, sec in enumerate(secs):
            for half in range(2):
                buf = wch[ci % 3]
                c0 = sec * 1024 + half * 512
                self.dma(buf[:], wm[:, :, c0:c0 + 512], w=[("wch", ci % 3)], eng="pool")
                for jb in range(4):
                    col = si * 8 + half * 4 + jb
                    for kc in range(8):
                        self.mm(ps[0][:, col:col + 1], buf[:, kc, jb * 128:(jb + 1) * 128], condb[:, kc:kc + 1],
                                start=(kc == 0), stop=(kc == 7), r=[("wch", ci % 3), "condb"], w=["psA"])
                ci += 1
        self.tt("dve", self.modfm[:], ps[0][:, 0:32], bmod[:], ALU.add, r=["psA", "bmod"], w=["modfm"])
        self.ts("dve", tmp8[:], self.modfm[:, 8:16], 1.0, None, ALU.add, r=["modfm"], w=["tmp8"])
        self.tt("dve", self.am[:], tmp8[:], gmix[:], ALU.mult, r=["tmp8", "gmix"], w=["am"])
        self.ts("dve", tmp8[:], self.modfm[:, 24:32], 1.0, None, ALU.add, r=["modfm"], w=["tmp8"])
        self.tt("dve", self.af[:], tmp8[:], gffn[:], ALU.mult, r=["tmp8", "gffn"], w=["af"])
        self.bm = self.modfm[:, 0:8]
        self.bfv = self.modfm[:, 16:24]
        self.S.fence()
        M.release(mk)

    def ht_fe1(self, x_rows, xt, xt_key, xn, xn_key, xn_eng="act"):
        c = self.stat_col()
        ssq = self.stat[:, 0, c:c + 1]
        ms = self.stat[:, 1, c:c + 1]
        rstd = self.stat[:, 2, c:c + 1]
        sk = ("st", c)
        if x_rows is not None:
            self.dma(xt[:], x_rows, w=[xt_key])
        self.stt("dve", xn[:], xt[:], 1.0, xt[:], ALU.mult, ALU.mult, r=[xt_key], w=[(sk, 0), xn_key], accum_out=ssq)
        self.ts("dve", ms, ssq, 1.0 / D, EPS, ALU.mult, ALU.add, r=[(sk, 0)], w=[(sk, 1)])
        self.tt("pool", rstd, ms, self.mhalf[:], ALU.pow, r=[(sk, 1), "mhalf"], w=[(sk, 2)])
        if xn_eng == "act":
            self.act(xn[:], xt[:], AF.Copy, r=[xt_key, (sk, 2)], w=[xn_key], scale=rstd)
        else:
            self.ts(xn_eng, xn[:], xt[:], rstd, None, ALU.mult, r=[xt_key, (sk, 2)], w=[xn_key])

    def ht_fe2(self, xn, xn_key, psbank, ps_key, hT, col0, hkey, a_sc, b_sc):
        pb = self.psb[psbank]
        for kc in range(8):
            self.tr(pb[:, kc * 128:(kc + 1) * 128], xn[:, kc * 128:(kc + 1) * 128], self.ident_b[:],
                    r=[xn_key, "ident_b"], w=[ps_key])
        for kc in range(8):
            self.ts("dve", hT[:, kc, col0:col0 + 128], pb[:, kc * 128:(kc + 1) * 128], a_sc[:, kc:kc + 1], b_sc[:, kc:kc + 1],
                    ALU.mult, ALU.add, r=[ps_key, "am", "af", "modfm"], w=[hkey])

    def make_ht_tile(self, x_rows, xt, xt_key, xn, xn_key, psbank, ps_key, hT, col0, hkey, a_sc, b_sc, junk, xn_eng="act"):
        self.ht_fe1(x_rows, xt, xt_key, xn, xn_key, xn_eng)
        self.ht_fe2(xn, xn_key, psbank, ps_key, hT, col0, hkey, a_sc, b_sc)

    def rope_tables(self, pos_ap, bufs):
        R = slice(64, 96)
        cosT, sinT, tA, tB = bufs["cosT"], bufs["sinT"], bufs["tA"], bufs["tB"]
        posi = tB[:].bitcast(I32)
        kc_, ks, ka, kb = [(bufs["k"], n) for n in ("cosT", "sinT", "tA", "tB")]
        kp = kb
        self.dma(posi[:, :], pos_ap.partition_broadcast(128), w=[kp])
        self.ts("dve", tA[R, :], posi[R, :], self.invf[R, 0:1], None, ALU.mult, r=[kp, "invf"], w=[ka])
        self.ts("dve", posi[R, :], tA[R, :], 1.0 / (2 * PI), None, ALU.mult, r=[ka], w=[kp])
        self.copy("dve", tB[R, :], posi[R, :], r=[kp], w=[kb])
        self.stt("dve", tA[R, :], tB[R, :], -2 * PI, tA[R, :], ALU.mult, ALU.add, r=[kb, ka], w=[ka])

        def wrap(dst, shift, dk):
            self.ts("dve", dst[R, :], tA[R, :], shift, None, ALU.add, r=[ka], w=[dk])
            self.tss("dve", tB[R, :], dst[R, :], PI, ALU.is_gt, r=[dk], w=[kb])
            self.stt("dve", dst[R, :], tB[R, :], -2 * PI, dst[R, :], ALU.mult, ALU.add, r=[kb, dk], w=[dk])
            self.tss("dve", tB[R, :], dst[R, :], -PI, ALU.is_lt, r=[dk], w=[kb])
            self.stt("dve", dst[R, :], tB[R, :], 2 * PI, dst[R, :], ALU.mult, ALU.add, r=[kb, dk], w=[dk])
            self.act(dst[R, :], dst[R, :], AF.Sin, r=[dk], w=[dk])

        wrap(sinT, 0.0, ks)
        wrap(cosT, PI / 2, kc_)
        return kc_, ks

    def phase_b(self):
        M, I, ps, psb = self.M, self.I, self.ps, self.psb
        self.kv_mark = M.mark()
        self.ktm = M.alloc("ktm", [128, 8, SEQ], BF16)
        self.vm = M.alloc("vm", [128, 32, 8, 65], BF16)
        self.oT_off = M.mark()
        self.oT = M.alloc("oT", [128, 8, 2048], BF16)
        self.oT_end = M.mark()
        M.release(self.oT_off)
        mk = M.mark()
        win = I["w_in"].rearrange("(kc p) n -> p kc n", p=128)
        wk_tok = M.alloc("wk_tok", [128, 8, 640], BF16)
        wk_fk = M.alloc("wk_fk", [128, 8, 512], BF16)
        wk_r = M.alloc("wk_r", [128, 8, 32], BF16)
        wk_rot = M.alloc("wk_rot", [128, 8, 32], BF16)
        wf3 = M.alloc("wf3", [128, 8, 72], BF16)
        wkv_k = M.alloc("wkv_k", [128, 8, 64], BF16)
        wkv_v = M.alloc("wkv_v", [128, 8, 64], BF16)
        self.dma(wk_tok[:, :, 0:128], win[:, :, 256:384], w=["wk_tok"], eng="pool")
        self.dma(wk_tok[:, :, 128:640], win[:, :, 1440:1952], w=["wk_tok"], eng="pool")
        self.dma(wk_fk[:], win[:, :, 928:1440], w=["wk_fk"], eng="pool")
        self.dma(wk_r[:], win[:, :, 384:416], w=["wk_r"], eng="pool")
        self.dma(wk_rot[:, :, 0:16], win[:, :, 400:416], w=["wk_rot"], eng="pool")
        self.dma(wk_rot[:, :, 16:32], win[:, :, 384:400], w=["wk_rot"], eng="pool")
        self.ts("dve", wk_rot[:, :, 0:16], wk_rot[:, :, 0:16], -1.0, None, ALU.mult, r=["wk_rot"], w=["wk_rot"])
        self.memset("dve", wf3[:], 0.0, w=["wf3"])
        for o3 in (0, 32, 64):
            self.dma(wf3[:, :, o3:o3 + 8], win[:, :, 1952:1960], w=["wf3"], eng="pool")
        wkv = I["w_kv_up"].rearrange("f (h c) -> f h c", c=128)
        self.dma(wkv_k[:], wkv[:, :, 0:64], w=["wkv_k"], eng="pool")
        self.dma(wkv_v[:], wkv[:, :, 64:128], w=["wkv_v"], eng="pool")
        self.memset("pool", self.vm[:, :, :, 64:65], 1.0, w=["vm_ones"])

        NX = 4
        xt = [M.alloc("xt%d" % i, [128, D], F32) for i in range(NX)]
        xn = [M.alloc("xn%d" % i, [128, D], BF16) for i in range(2)]
        hT = [M.alloc("hT%d" % i, [128, 8, 512], BF16) for i in range(2)]
        kvl = [M.alloc("kvl%d" % i, [128, 128], F32) for i in range(2)]
        kvn = [M.alloc("kvn%d" % i, [128, 128], BF16) for i in range(2)]
        kvnT = [M.alloc("kvnT%d" % i, [128, 512], BF16) for i in range(2)]
        kfs = M.alloc("kfs", [128, 4, 512], BF16)
        vfs = [M.alloc("vfs%d" % i_, [128, 4, 8, 65], BF16) for i_ in range(2)]
        fT = M.alloc("fT", [128, SEQ], F32)
        rb = {"cosT": M.alloc("cosT", [128, 512], F32),
              "sinT": M.alloc("sinT", [128, 512], F32), "tA": M.alloc("tA", [128, 512], F32),
              "tB": M.alloc("tB", [128, 512], F32), "k": "rbB"}
        for v_ in vfs:
            self.memset("pool", v_[:, :, :, 64:65], 1.0, w=["vfs"])
        self.memset("dve", fT[:], 0.0, w=["fT"])
        fm_i = 0

        def xload(i):
            if i < 32:
                self.dma(xt[i % NX][:], I["x_all"][i * 128:(i + 1) * 128, :], w=[("xt", i % NX)])

        def fe1(i):
            if i < 32:
                self.ht_fe1(None, xt[i % NX], ("xt", i % NX), xn[i % 2], ("xn", i % 2))

        def fe2(i):
            fe2_pe(i)
            fe2_dve(i)

        def fe2_pe(i):
            if i < 32:
                pb = self.psb[i % 2]
                for kc in range(8):
                    self.tr(pb[:, kc * 128:(kc + 1) * 128], xn[i % 2][:, kc * 128:(kc + 1) * 128], self.ident_b[:],
                            r=[("xn", i % 2), "ident_b"], w=[("ps", i % 2)])

        def fe2_dve(i):
            if i < 32:
                pb = self.psb[i % 2]
                H_ = hT[(i // 4) % 2]
                col0 = (i % 4) * 128
                for kc in range(8):
                    self.ts("dve", H_[:, kc, col0:col0 + 128], pb[:, kc * 128:(kc + 1) * 128], self.am[:, kc:kc + 1], self.bm[:, kc:kc + 1],
                            ALU.mult, ALU.add, r=[("ps", i % 2), "am", "modfm"], w=[("hT", (i // 4) % 2, i % 4)])

        st = {"fm_i": 0, "rope": {}}

        def be1_pe(i):
            g, t = i // 4, i % 4
            hb = g % 2
            H = hT[hb]
            tcs = slice(t * 128, (t + 1) * 128)
            hk = [("hT", hb, t)]
            pa = 2 + (i % 2)
            for kc in range(8):
                self.mm(ps[pa][:, :], H[:, kc, tcs], wk_tok[:, kc, 128:640], start=(kc == 0), stop=(kc == 7),
                        r=hk + ["wk_tok"], w=[("ps", pa)])
            for kc in range(8):
                self.mm(ps[4][:, t * 128:(t + 1) * 128], H[:, kc, tcs], wk_tok[:, kc, 0:128], start=(kc == 0), stop=(kc == 7),
                        r=hk + ["wk_tok"], w=[("ps4", t)])

        def be1_rest(i):
            g, t = i // 4, i % 4
            pa = 2 + (i % 2)
            self.copy("act", vfs[g % 2][:, t, :, 0:64], ps[pa][:, :].rearrange("p (h c) -> p h c", c=64), r=[("ps", pa)], w=["vfs"])
            c = self.stat_col()
            st["kvc"] = c
            ssq, ms, rstd = self.stat[:, 0, c:c + 1], self.stat[:, 1, c:c + 1], self.stat[:, 2, c:c + 1]
            sk = ("st", c)
            kb = i % 2
            self.copy("dve", kvl[kb][:], ps[4][:, t * 128:(t + 1) * 128], r=[("ps4", t)], w=[("kvl", kb)])
            self.stt("dve", kvn[kb][:], kvl[kb][:], 1.0, kvl[kb][:], ALU.mult, ALU.mult, r=[("kvl", kb)], w=[(sk, 0), ("kvn", kb)], accum_out=ssq)
            self.ts("dve", ms, ssq, 1.0 / 128, EPS, ALU.mult, ALU.add, r=[(sk, 0)], w=[(sk, 1)])
            self.tt("pool", rstd, ms, self.mhalf[:], ALU.pow, r=[(sk, 1), "mhalf"], w=[(sk, 2)])

        def be1_tail(i):
            c = st["kvc"]
            rstd = self.stat[:, 2, c:c + 1]
            sk = ("st", c)
            kb = i % 2
            self.ts("dve", kvn[kb][:], kvl[kb][:], rstd, None, ALU.mult, r=[("kvl", kb), (sk, 2)], w=[("kvn", kb)])

        def be2(i):
            g, t = i // 4, i % 4
            hb = g % 2
            tcs = slice(t * 128, (t + 1) * 128)
            kb = i % 2
            pa = 2 + (i % 2)
            self.tr(psb[5][:, t * 128:(t + 1) * 128], kvn[kb][:], self.ident_b[:], r=[("kvn", kb), "ident_b"], w=[("ps5", t)])
            self.ts("dve", kvnT[hb][:, tcs], psb[5][:, t * 128:(t + 1) * 128], self.gkv[:, 0:1], None, ALU.mult,
                    r=[("ps5", t), "gkv"], w=[("kvnT", hb, t)])
            self.mm(ps[pa][:, :], kvnT[hb][:, tcs], wkv_v[:].rearrange("p h c -> p (h c)"), r=[("kvnT", hb, t), "wkv_v"], w=[("ps", pa)])
            self.copy("act", self.vm[:, i, :, 0:64], ps[pa][:, :].rearrange("p (h c) -> p h c", c=64), r=[("ps", pa)], w=[("vm", i)])

        def grp(g):
            hb = g % 2
            H = hT[hb]
            gc = slice(g * 512, (g + 1) * 512)
            kcos, ksin = st["rope"][g]
            hk = [("hT", hb, t) for t in range(4)]
            kvk = [("kvnT", hb, t) for t in range(4)]
            for pr in range(4):
                pf = 6 + (st["fm_i"] % 2); st["fm_i"] += 1
                for kc in range(8):
                    self.mm(ps[pf][:, :], wk_fk[:, kc, pr * 128:(pr + 1) * 128], H[:, kc, :], start=(kc == 0), stop=(kc == 7),
                            r=hk + ["wk_fk"], w=[("ps", pf)])
                self.copy("act" if pr % 2 == 0 else "dve", kfs[:, pr, :], ps[pf][:, :], r=[("ps", pf)], w=["kfs"])
            for h in range(8):
                pf = 6 + (st["fm_i"] % 2); st["fm_i"] += 1
                self.mm(ps[pf][0:64, :], wkv_k[:, h, :], kvnT[hb][:, :], r=kvk + ["wkv_k"], w=[("ps", pf)])
                self.copy("act" if h % 2 == 0 else "dve", self.ktm[0:64, h, gc], ps[pf][0:64, :], r=[("ps", pf)], w=[("ktm", g)])
            for kc in range(8):
                self.mm(ps[6][64:96, :], wk_r[:, kc, :], H[:, kc, :], start=(kc == 0), stop=(kc == 7), r=hk + ["wk_r"], w=[("ps", 6)])
            for kc in range(8):
                self.mm(ps[7][64:96, :], wk_rot[:, kc, :], H[:, kc, :], start=(kc == 0), stop=(kc == 7), r=hk + ["wk_rot"], w=[("ps", 7)])
            st["fm_i"] += 2
            R = slice(64, 96)
            self.tt("dve", rb["tA"][R, :], ps[6][R, :], rb["cosT"][R, :], ALU.mult, r=[("ps", 6), kcos], w=[("rbB", "tA")])
            self.tt("dve", rb["tB"][R, :], ps[7][R, :], rb["sinT"][R, :], ALU.mult, r=[("ps", 7), ksin], w=[("rbB", "tB")])
            self.tt("dve", self.ktm[R, 0, gc], rb["tA"][R, :], rb["tB"][R, :], ALU.add, r=[("rbB", "tA"), ("rbB", "tB")], w=[("ktm", g)])
            for h in range(1, 8):
                self.copy("act" if h % 2 == 1 else "pool", self.ktm[R, h, gc], self.ktm[R, 0, gc], r=[("ktm", g)], w=[("ktmr", g, h)])
            pf = 6 + (st["fm_i"] % 2); st["fm_i"] += 1
            for kc in range(8):
                self.mm(ps[pf][0:72, :], wf3[:, kc, :], H[:, kc, :], start=(kc == 0), stop=(kc == 7), r=hk + ["wf3"], w=[("ps", pf)])
            self.act(fT[0:72, gc], ps[pf][0:72, :], AF.Exp, r=[("ps", pf), "negbf"], w=[("fT", g)], scale=-1.0, bias=self.negbf[0:72, 0:1])
            self.act(fT[0:72, gc], fT[0:72, gc], AF.Ln, r=[("fT", g)], w=[("fT", g)], bias=1.0)
            self.dma(self.scr_kf[:, :, gc], kfs[:], r=["kfs"], w=[("scr_kf", g)])
            self.dma(self.scr_vf[:, 4 * g:4 * g + 4, :], vfs[g % 2][:].rearrange("p t h c -> p t (h c)"), r=["vfs"], w=[("scr_vf", g)])

        xload(0)
        xload(1)
        xload(2)
        fe1(0)
        fe1(1)
        fe2(0)
        for i in range(32):
            g, t = i // 4, i % 4
            xload(i + 3)
            fe2_pe(i + 1)
            be1_pe(i)
            fe2_dve(i + 1)
            be1_rest(i)
            fe1(i + 2)
            be1_tail(i)
            if t == 1:
                st["rope"][g] = self.rope_tables(I["pos_all"][g * 512:(g + 1) * 512], rb)
            if i >= 1:
                be2(i - 1)
                if (i - 1) % 4 == 3:
                    grp((i - 1) // 4)
        be2(31)
        grp(7)
        fkeys = [("fT", g) for g in range(8)]
        self.S.op("dve", lambda e: e.tensor_tensor_scan(out=fT[0:72, :], data0=fT[0:72, :], data1=fT[0:72, :], initial=0.0,
                                                         op0=ALU.add, op1=ALU.max), fkeys, ["fTs"])
        for i in range(32):
            self.tr(ps[0][:, i * 8:(i + 1) * 8], fT[0:8, i * 128:(i + 1) * 128], self.ident_f[0:8, 0:8], r=["fTs", "ident_f"], w=[("ps", 0)])
        self.copy("dve", self.fkT[:].rearrange("p t h -> p (t h)"), ps[0][:, 0:256], r=[("ps", 0)], w=["fkT"])
        self.ts("dve", self.pneg[:], self.pvec[:, 1:2], -1e30, None, ALU.mult, r=["pvec"], w=["pneg"])
        self.ts("dve", self.fkT2[:].rearrange("p t h -> p (t h)"), self.fkT[:].rearrange("p t h -> p (t h)"), self.pneg[:, 0:1], None,
                ALU.add, r=["fkT", "pneg"], w=["fkT2"])
        fv4 = fT[0:72, :].rearrange("p (j two r) -> p j two r", two=2, r=128)
        for hf in range(2):
            js = slice(hf * 8, hf * 8 + 8)
            ev = fv4[:, js, 0, :]
            od = fv4[:, js, 1, :]
            T0 = xt[0][0:72, :].rearrange("p (j r) -> p j r", r=128)
            T1 = xt[1][0:72, :].rearrange("p (j r) -> p j r", r=128)
            self.ts("dve", T0, ev, self.pvec[0:72, 1:2], None, ALU.mult, r=["fTs", "pvec", ("xt", 0)], w=[("xt", 0)])
            self.stt("dve", T0, od, self.pvec[0:72, 0:1], T0, ALU.mult, ALU.add, r=["fTs", "pvec", ("xt", 0)], w=[("xt", 0)])
            self.ts("dve", xt[0][0:72, :], xt[0][0:72, :], -1.0, None, ALU.mult, r=[("xt", 0)], w=[("xt", 0)])
            hi = xn[0]; mid = xn[1]
            cs = slice(hf * 1024, (hf + 1) * 1024)
            self.copy("dve", hi[0:72, :], xt[0][0:72, :], r=[("xt", 0)], w=[("xn", 0)])
            self.tt("dve", xt[1][0:72, :], xt[0][0:72, :], hi[0:72, :], ALU.subtract, r=[("xt", 0), ("xn", 0)], w=[("xt", 1)])
            self.copy("dve", mid[0:72, :], xt[1][0:72, :], r=[("xt", 1)], w=[("xn", 1)])
            self.tt("dve", xt[2][0:72, :], xt[1][0:72, :], mid[0:72, :], ALU.subtract, r=[("xt", 1), ("xn", 1)], w=[("xt", 2)])
            self.copy("dve", self.f3[0:8, cs], hi[0:8, :], r=[("xn", 0), "f3"], w=["f3"])
            self.copy("dve", self.f3[32:40, cs], mid[32:40, :], r=[("xn", 1), "f3"], w=["f3"])
            self.copy("dve", self.f3[64:72, cs], xt[2][64:72, :], r=[("xt", 2), "f3"], w=["f3"])
        if self.debug:
            d1 = self.dbg_out("dbg_ktm", [128, 8, SEQ], BF16)
            self.dma(d1, self.ktm[:], r=[("ktm", g) for g in range(8)] + [("ktmr", g, h) for g in range(8) for h in range(1, 8)])
            d2 = self.dbg_out("dbg_vm", [128, 32, 520], BF16)
            self.dma(d2, self.vm[:].rearrange("p t h c -> p t (h c)"), r=[("vm", i) for i in range(32)] + ["vm_ones"])
            d3 = self.dbg_out("dbg_fkT", [128, 256], F32)
            self.dma(d3, self.fkT[:].rearrange("p t h -> p (t h)"), r=["fkT"])
            d4 = self.dbg_out("dbg_f3", [128, 2048], BF16)
            self.dma(d4, self.f3[:], r=["f3"])
        self.S.fence()
        M.release(mk)

    def qside_common_alloc(self):
        M = self.M
        self.q_xt = [M.alloc("qxt%d" % i, [128, D], F32) for i in range(1)]
        self.q_xn = M.alloc("qxn", [128, D], BF16)
        self.q_hT = M.alloc("qhT", [128, 8, 512], BF16)
        self.pbuf = [M.alloc("pbuf%d" % i, [128, 512], BF16) for i in range(4)]
        self.otok = M.alloc("otok", [128, 4, 512], BF16)
        self.rc = M.alloc("rc", [128, 8], F32)

    def qside_gen(self, g, fox, W):
        I, ps, psb = self.I, self.ps, self.psb
        H = self.q_hT
        qb = g % 2
        for t in range(4):
            i = 4 * g + t
            xs = 0
            self.ht_fe1(I["x_own"][i * 128:(i + 1) * 128, :], self.q_xt[xs], ("qxt", xs), self.q_xn, "qxn", xn_eng="dve")
            yield
            yield
            self.ht_fe2(self.q_xn, "qxn", 5, ("ps", 5), H, t * 128, ("qhT", t), self.am, self.bm)
            yield
            yield
            if not fox:
                tcs = slice(t * 128, (t + 1) * 128)
                for kc in range(8):
                    self.mm(ps[6][:, 0:256], H[:, kc, tcs], W["w_ql"][:, kc, :], start=(kc == 0), stop=(kc == 7),
                            r=[("qhT", t), "w_ql"], w=[("ps6", "a")])
                yield
                c = self.stat_col()
                ssq, ms, rstd = self.stat[:, 0, c:c + 1], self.stat[:, 1, c:c + 1], self.stat[:, 2, c:c + 1]
                sk = ("st", c)
                self.copy("dve", W["ql"][:], ps[6][:, 0:256], r=[("ps6", "a")], w=["ql"])
                self.stt("dve", W["qn"][:], W["ql"][:], 1.0, W["ql"][:], ALU.mult, ALU.mult, r=["ql"], w=[(sk, 0), "qn"], accum_out=ssq)
                self.ts("dve", ms, ssq, 1.0 / 256, EPS, ALU.mult, ALU.add, r=[(sk, 0)], w=[(sk, 1)])
                self.tt("pool", rstd, ms, self.mhalf[:], ALU.pow, r=[(sk, 1), "mhalf"], w=[(sk, 2)])
                self.ts("dve", W["qn"][:], W["ql"][:], rstd, None, ALU.mult, r=["ql", (sk, 2)], w=["qn"])
                yield
                yield
                for kc in range(2):
                    self.tr(psb[6][:, 512 + kc * 128:512 + (kc + 1) * 128], W["qn"][:, kc * 128:(kc + 1) * 128], self.ident_b[:],
                            r=["qn", "ident_b"], w=[("ps6", "b")])
                for kc in range(2):
                    self.ts("dve", W["qnT"][:, kc, tcs], psb[6][:, 512 + kc * 128:512 + (kc + 1) * 128], self.gq[:, kc:kc + 1], None,
                            ALU.mult, r=[("ps6", "b"), "gq"], w=[("qnT", t)])
                yield
        hk = [("qhT", t) for t in range(4)]
        if fox:
            QT = W["qtf"][qb]
            for h in range(8):
                for kc in range(8):
                    self.mm(ps[7][0:64, :], W["w_fq"][:, kc, h * 64:(h + 1) * 64], H[:, kc, :], start=(kc == 0), stop=(kc == 7),
                            r=hk + ["w_fq"], w=[("ps", 7)])
                self.mm(ps[6][64:67, :], self.sel3[0:72, h, :], self.f3[0:72, g * 512:(g + 1) * 512], r=["sel3", "f3"], w=[("ps", 6)])
                yield
                self.copy("dve", QT[0:64, h, :], ps[7][0:64, :], r=[("ps", 7)], w=[("qtf", qb)])
                self.copy("dve", QT[64:67, h, :], ps[6][64:67, :], r=[("ps", 6)], w=[("qtf", qb)])
                yield
        else:
            QT = W["qtm"][qb]
            rbq = W["rbq"]
            kcos, ksin = self.rope_tables(I["pos_own"][g * 512:(g + 1) * 512], rbq)
            yield
            qk = [("qnT", t) for t in range(4)]
            R = slice(64, 96)
            for h in range(8):
                for kc in range(2):
                    self.mm(ps[7][0:96, :], W["wq_up"][:, kc, h * 96:(h + 1) * 96], W["qnT"][:, kc, :], start=(kc == 0), stop=(kc == 1),
                            r=qk + ["wq_up"], w=[("ps", 7)])
                for kc in range(2):
                    self.mm(ps[5][64:96, 0:512], W["wq_rot"][:, kc, h, :], W["qnT"][:, kc, :], start=(kc == 0), stop=(kc == 1),
                            r=qk + ["wq_rot"], w=[("ps", 5)])
                yield
                self.copy("dve", QT[0:64, h, :], ps[7][0:64, :], r=[("ps", 7)], w=[("qtm", qb)])
                self.tt("dve", rbq["tA"][R, :], ps[7][R, :], rbq["cosT"][R, :], ALU.mult, r=[("ps", 7), kcos], w=[("rbq", "tA")])
                self.tt("dve", rbq["tB"][R, :], ps[5][R, 0:512], rbq["sinT"][R, :], ALU.mult, r=[("ps", 5), ksin], w=[("rbq", "tB")])
                self.tt("dve", QT[R, h, :], rbq["tA"][R, :], rbq["tB"][R, :], ALU.add, r=[("rbq", "tA"), ("rbq", "tB")], w=[("qtm", qb)])
                yield

    def attn_group(self, g, fox, W, side_gen, extra_gen=None):
        ps, psb = self.ps, self.psb
        qb = g % 2
        nkb = 8 * g + 8
        tiles = []
        for h in range(8):
            for n in range(nkb):
                if n < 8 * g:
                    tiles.append((h, n, 0, None))
                else:
                    m = n - 8 * g
                    tiles.append((h, n, (m // 2) * 128, self.m0 if m % 2 == 0 else self.m1))
        NT = len(tiles)
        base = 4 if fox else 0
        sc = SC_FOX if fox else SC_MLA

        def qk(i):
            h, n, c0, _ = tiles[i]
            bk = i % 3
            ncs = slice(n * 128, (n + 1) * 128)
            if fox:
                self.mm(ps[bk][:, c0:512], W["ktf"][0:67, h, ncs], W["qtf"][qb][0:67, h, c0:512], start=True, stop=True,
                        r=[("qtf", qb), "ktf"], w=[("ps", bk)])
            else:
                self.mm(ps[bk][:, c0:512], self.ktm[0:96, h, ncs], W["qtm"][qb][0:96, h, c0:512], start=True, stop=True,
                        r=[("qtm", qb)], w=[("ps", bk)])

        def expo(i):
            h, n, c0, mk = tiles[i]
            bk = i % 3
            pb = i % 4
            P_ = self.pbuf[pb]
            if fox and mk is self.m1:
                self.act(P_[:, c0:c0 + 128], ps[bk][:, c0:c0 + 128], AF.Exp, r=[("ps", bk), "fkT2"], w=[("pbuf", pb)], scale=sc,
                         bias=self.fkT2[:, n, h:h + 1])
                if c0 + 128 < 512:
                    self.act(P_[:, c0 + 128:512], ps[bk][:, c0 + 128:512], AF.Exp, r=[("ps", bk), "fkT"], w=[("pbuf", pb)], scale=sc,
                             bias=self.fkT[:, n, h:h + 1])
            elif fox:
                self.act(P_[:, c0:512], ps[bk][:, c0:512], AF.Exp, r=[("ps", bk), "fkT"], w=[("pbuf", pb)], scale=sc,
                         bias=self.fkT[:, n, h:h + 1])
            else:
                self.act(P_[:, c0:512], ps[bk][:, c0:512], AF.Exp, r=[("ps", bk)], w=[("pbuf", pb)], scale=sc)
            if mk is not None:
                self.tt("pool", P_[:, c0:c0 + 128], P_[:, c0:c0 + 128], mk[:], ALU.mult, r=[("pbuf", pb), "m0", "m1"], w=[("pbuf", pb)])

        def pv(i):
            h, n, c0, _ = tiles[i]
            pb = i % 4
            ob = 3 + (h % 2)
            P_ = self.pbuf[pb]
            V = W["vf"] if fox else self.vm
            for jj in range(c0 // 128, 4):
                first = (n == 0 and jj == 0)
                last = (n == nkb - 1 and jj == 3)
                self.mm(ps[ob][:, jj * 65:(jj + 1) * 65], P_[:, jj * 128:(jj + 1) * 128], V[:, n, h, :], start=first, stop=last,
                        r=[("pbuf", pb), "vf"], w=[("ps", ob)], sgc=True)
            if n == nkb - 1:
                o4 = ps[ob][:, 0:260].rearrange("p (j c) -> p j c", c=65)
                self.S.op("dve", lambda e: e.reciprocal(out=self.rc[:, 0:4], in_=o4[:, :, 64]), [("ps", ob)], ["rc"])
                for jj in range(4):
                    self.ts("dve", self.otok[:, jj, h * 64:(h + 1) * 64], ps[ob][:, jj * 65:jj * 65 + 64], self.rc[:, jj:jj + 1], None,
                            ALU.mult, r=[("ps", ob), "rc"], w=[("otok", h)])

        side_every = max(1, NT // (48 if fox else 64))
        qk(0)
        if NT > 1:
            qk(1)
        for i in range(NT):
            expo(i)
            if i + 2 < NT:
                qk(i + 2)
            pv(i)
            if i % side_every == side_every - 1:
                if side_gen is not None:
                    next(side_gen, None)
            if extra_gen is not None and i % 6 == 5:
                next(extra_gen, None)
        if side_gen is not None:
            for _ in side_gen:
                pass
        for rnd in range(2):
            for jl in range(2):
                jj = rnd * 2 + jl
                for c in range(4):
                    self.tr(psb[5][:, (jl * 4 + c) * 128:(jl * 4 + c + 1) * 128], self.otok[:, jj, c * 128:(c + 1) * 128], self.ident_b[:],
                            r=[("otok", hh) for hh in range(8)] + ["ident_b"], w=[("ps", 5)])
            for jl in range(2):
                jj = rnd * 2 + jl
                t0 = g * 512 + jj * 128
                self.copy("dve", self.oT[:, base:base + 4, t0:t0 + 128],
                          psb[5][:, jl * 512:(jl + 1) * 512].rearrange("p (c t) -> p c t", t=128), r=[("ps", 5)], w=[("oT", base, g)])

    def moe_precast_gen(self):
        I = self.I
        for e in range(N_EXP):
            rows = slice(e * 128, (e + 1) * 128)
            gu = self.wall_s[rows, 0:4096].rearrange("p (kc n) -> p kc n", n=512)
            self.dma(gu[:, :, 0:256], I["w_exp_gate"][e].rearrange("(kc p) n -> p kc n", p=128), w=[("wgu_s", e, 0)], eng="pool")
            yield
            self.dma(gu[:, :, 256:512], I["w_exp_up"][e].rearrange("(kc p) n -> p kc n", p=128), w=[("wgu_s", e, 1)], eng="pool")
            yield
            self.dma(self.wall_s[rows, 4096:6144].rearrange("p (fc n) -> p fc n", n=D), I["w_exp_down"][e].rearrange("(fc p) n -> p fc n", p=128),
                     w=[("wd_s", e)], eng="pool")
            yield

    def pass_mla(self):
        M, I = self.M, self.I
        M.release(self.oT_end)
        mk = M.mark()
        self.qside_common_alloc()
        W = {}
        W["w_ql"] = M.alloc("w_ql", [128, 8, 256], BF16)
        W["wq_up"] = M.alloc("wq_up", [128, 2, 768], BF16)
        W["wq_rot"] = M.alloc("wq_rot", [128, 2, 8, 32], BF16)
        W["ql"] = M.alloc("ql", [128, 256], F32)
        W["qn"] = M.alloc("qn", [128, 256], BF16)
        W["qnT"] = M.alloc("qnT", [128, 2, 512], BF16)
        W["qtm"] = [M.alloc("qtm%d" % i, [128, 8, 512], BF16) for i in range(2)]
        W["rbq"] = {"cosT": M.alloc("qcosT", [128, 512], F32),
                    "sinT": M.alloc("qsinT", [128, 512], F32), "tA": M.alloc("qtA", [128, 512], F32),
                    "tB": M.alloc("qtB", [128, 512], F32), "k": "rbq"}
        win = I["w_in"].rearrange("(kc p) n -> p kc n", p=128)
        self.dma(W["w_ql"][:], win[:, :, 0:256], w=["w_ql"], eng="pool")
        wq = I["w_q_up"].rearrange("(kc p) n -> p kc n", p=128)
        self.dma(W["wq_up"][:], wq, w=["wq_up"], eng="pool")
        wq4 = I["w_q_up"].rearrange("(kc p) (h c) -> p kc h c", p=128, c=96)
        for kc in range(2):
            self.dma(W["wq_rot"][:, kc, :, 0:16], wq4[:, kc, :, 80:96], w=["wq_rot"], eng="pool")
            self.dma(W["wq_rot"][:, kc, :, 16:32], wq4[:, kc, :, 64:80], w=["wq_rot"], eng="pool")
        self.ts("dve", W["wq_rot"][:, :, :, 0:16], W["wq_rot"][:, :, :, 0:16], -1.0, None, ALU.mult, r=["wq_rot"], w=["wq_rot"])
        for _ in self.qside_gen(0, False, W):
            pass
        for g in range(4):
            gen = self.qside_gen(g + 1, False, W) if g < 3 else None
            self.attn_group(g, False, W, gen, self.precast)
        if self.debug:
            d = self.dbg_out("dbg_oT_mla", [128, 4, 2048], BF16)
            self.dma(d, self.oT[:, 0:4, :], r=[("oT", 0, g) for g in range(4)])
        self.S.fence()
        M.release(mk)

    def pass_fox(self):
        M, I = self.M, self.I
        M.release(self.kv_mark)
        self.attn_alloc_common_after = None
        W = {}
        W["ktf"] = M.alloc("ktf", [128, 8, SEQ], BF16)
        W["vf"] = M.alloc("vf", [128, 32, 8, 65], BF16)
        assert M.mark() <= self.oT_off
        M.release(self.oT_end)
        mk = M.mark()
        self.qside_common_alloc()
        W["w_fq"] = M.alloc("w_fq", [128, 8, 512], BF16)
        W["qtf"] = [M.alloc("qtf%d" % i, [128, 8, 512], BF16) for i in range(2)]
        win = I["w_in"].rearrange("(kc p) n -> p kc n", p=128)
        self.dma(W["w_fq"][:], win[:, :, 416:928], w=["w_fq"], eng="pool")
        for pr in range(4):
            for hf in range(2):
                self.dma(W["ktf"][0:64, 2 * pr + hf, :], self.scr_kf[hf * 64:(hf + 1) * 64, pr, :], r=[("scr_kf", gg) for gg in range(8)], w=["ktf"])
        self.memset("dve", W["ktf"][64:67, :, :], 8.0, w=["ktf"])
        for q in range(4):
            ts_ = slice(q * 8, (q + 1) * 8)
            self.dma(W["vf"][:, ts_, :, :].rearrange("p t h c -> p t (h c)"), self.scr_vf[:, ts_, :],
                     r=[("scr_vf", gg) for gg in range(8)], w=["vf"])
        for _ in self.qside_gen(0, True, W):
            pass
        for g in range(4):
            gen = self.qside_gen(g + 1, True, W) if g < 3 else None
            self.attn_group(g, True, W, gen, self.precast)
        if self.precast is not None:
            for _ in self.precast:
                pass
        if self.debug:
            d = self.dbg_out("dbg_oT_fox", [128, 4, 2048], BF16)
            self.dma(d, self.oT[:, 4:8, :], r=[("oT", 4, g) for g in range(4)])
        self.S.fence()
        M.release(mk)
        self.post_mark = self.kv_mark

    def bc3(self, ap2, n):
        return ap2.unsqueeze(2).to_broadcast([128, ap2.shape[1], n])

    def phase_d(self):
        M, I, ps, psb = self.M, self.I, self.ps, self.psb
        M.release(self.kv_mark)
        self.gm_bc = M.alloc("gm_bc", [128, D], F32)
        self.gf_bc = M.alloc("gf_bc", [128, D], F32)
        self.h2T = [M.alloc("h2T_a", [128, 8, 1024], BF16), None]
        wr = M.alloc("wr", [128, 8, 64], BF16)
        brt = M.alloc("brt", [128, 64], F32)
        self.sel_bf = M.alloc("sel_bf", [128, 16, 64], BF16)
        self.dest6 = M.alloc("dest6", [128, 16, 8], U32)
        self.gate6 = M.alloc("gate6", [128, 16, 8], F32)
        self.widx = M.alloc("widx", [128, NBLK], U32)
        self.e_mark1 = M.mark()
        wg = M.alloc("wg", [128, 8, 2048], BF16)
        wo_m = M.alloc("wo_m", [128, 4, D], BF16)
        wo_f = M.alloc("wo_f", [128, 4, D], BF16)
        wout = M.alloc("wout", [128, 8, D], BF16)
        assert M.mark() <= self.oT_off, M.mark()
        M.release(self.oT_end)
        self.h2T[1] = M.alloc("h2T_b", [128, 8, 1024], BF16)
        self.e_mark2 = M.mark()
        mk2 = M.mark()
        cbc = M.alloc("cbc", [128, 8, 128], F32)
        ones = M.alloc("ones", [128, 128], F32)
        wch = [M.alloc("wchd%d" % i, [128, 8, 512], F32) for i in range(2)]
        self.memset("dve", ones[:], 1.0, w=["ones"])
        for kc in range(8):
            self.ts("dve", cbc[:, kc, :], ones[:], self.cond[:, kc:kc + 1], None, ALU.mult, r=["ones", "cond"], w=["cbc"])
        self.dma(self.gm_bc[:], I["bmod_g"][0].partition_broadcast(128), w=["gm_bc"])
        self.dma(self.gf_bc[:], I["bmod_g"][1].partition_broadcast(128), w=["gf_bc"])
        wm = I["w_mod"].rearrange("(kc p) n -> p kc n", p=128)
        ci = 0
        for sec, dst, dk in ((2, self.gm_bc, "gm_bc"), (5, self.gf_bc, "gf_bc")):
            for half in range(2):
                buf = wch[ci % 2]
                c0 = sec * 1024 + half * 512
                self.dma(buf[:], wm[:, :, c0:c0 + 512], w=[("wchd", ci % 2)])
                pb = ci % 2
                for kc in range(8):
                    self.mm(ps[pb][:, :], cbc[:, kc, :], buf[:, kc, :], start=(kc == 0), stop=(kc == 7),
                            r=["cbc", ("wchd", ci % 2)], w=[("ps", pb)])
                self.tt("dve", dst[:, half * 512:(half + 1) * 512], ps[pb][:, :], dst[:, half * 512:(half + 1) * 512], ALU.add,
                        r=[("ps", pb), dk], w=[dk])
                ci += 1
        win = I["w_in"].rearrange("(kc p) n -> p kc n", p=128)
        self.dma(wg[:, :, 0:1024], win[:, :, 1960:2984], w=["wg"], eng="pool")
        self.dma(wg[:, :, 1024:2048], win[:, :, 2984:4008], w=["wg"], eng="pool")
        self.dma(wo_m[:], I["w_o_mla"].rearrange("(c p) n -> p c n", p=128), w=["wo_m"], eng="pool")
        self.dma(wo_f[:], I["w_o_fox"].rearrange("(c p) n -> p c n", p=128), w=["wo_f"], eng="pool")
        self.dma(wout[:], I["w_out"].rearrange("(kc p) n -> p kc n", p=128), w=["wout"], eng="pool")
        self.dma(wr[:], I["w_router"].rearrange("(kc p) n -> p kc n", p=128), w=["wr"], eng="pool")
        self.dma(brt[:], I["b_router"].partition_broadcast(128), w=["brt"])
        self.S.fence()
        M.release(mk2)
        rt = {n: M.alloc("r_" + n, [128, 64], F32) for n in ("sc", "ch", "tmp", "mc", "sel")}
        self.gates = M.alloc("gates", [128, 16, 64], F32)
        r8 = {n: M.alloc("r8_" + n, [128, 8], F32) for n in ("m1", "m2", "gs", "top", "keep", "pen", "top6", "gsum")}
        mk3 = M.mark()
        xt = [M.alloc("dxt%d" % i, [128, D], F32) for i in range(4)]
        xn = M.alloc("dxn", [128, D], BF16)
        hT = M.alloc("dhT", [128, 8, 512], BF16)
        sa = M.alloc("sa", [128, 512], F32)
        sb_ = M.alloc("sb", [128, 512], F32)
        t1 = M.alloc("t1", [128, 512], F32)
        t2 = M.alloc("t2", [128, 512], F32)
        mT = M.alloc("mT", [128, 8, 512], BF16)
        for g in range(4):
            gcs = slice(g * 512, (g + 1) * 512)
            for t in range(4):
                i = 4 * g + t
                self.make_ht_tile(I["x_own"][i * 128:(i + 1) * 128, :], xt[t], ("dxt", t), xn, "dxn",
                                  6, ("ps", 6), hT, t * 128, ("dhT", t), self.am, self.bm, None)
            hk = [("dhT", t) for t in range(4)]
            ok = [("oT", 0, g), ("oT", 4, g)]
            for mc in range(8):
                b0 = 0 if mc % 2 == 0 else 2
                ms_ = slice(mc * 128, (mc + 1) * 128)
                for kc in range(8):
                    self.mm(ps[b0][:, :], wg[:, kc, ms_], hT[:, kc, :], start=(kc == 0), stop=(kc == 7), r=hk + ["wg"], w=[("ps", b0)])
                for kc in range(8):
                    self.mm(ps[b0 + 1][:, :], wg[:, kc, 1024 + mc * 128:1024 + (mc + 1) * 128], hT[:, kc, :], start=(kc == 0), stop=(kc == 7),
                            r=hk + ["wg"], w=[("ps", b0 + 1)])
                for c in range(4):
                    self.mm(ps[4][:, :], wo_m[:, c, ms_], self.oT[:, c, gcs], start=(c == 0), stop=(c == 3), r=ok + ["wo_m"], w=[("ps", 4)])
                for c in range(4):
                    self.mm(ps[5][:, :], wo_f[:, c, ms_], self.oT[:, 4 + c, gcs], start=(c == 0), stop=(c == 3), r=ok + ["wo_f"], w=[("ps", 5)])
                self.act(sa[:], ps[b0][:, :], AF.Sigmoid, r=[("ps", b0)], w=["sa"])
                self.act(sb_[:], ps[b0 + 1][:, :], AF.Sigmoid, r=[("ps", b0 + 1)], w=["sb"])
                self.tt("dve", t1[:], ps[4][:, :], sa[:], ALU.mult, r=[("ps", 4), "sa"], w=["t1"])
                self.tt("dve", t2[:], ps[5][:, :], sb_[:], ALU.mult, r=[("ps", 5), "sb"], w=["t2"])
                self.tt("pool", mT[:, mc, :], t1[:], t2[:], ALU.add, r=["t1", "t2"], w=[("mT", mc)])
            mk_ = [("mT", mc) for mc in range(8)]
            for t in range(4):
                i = 4 * g + t
                tcs = slice(t * 128, (t + 1) * 128)
                for half in range(2):
                    pb = 6 + half
                    hs = slice(half * 512, (half + 1) * 512)
                    for kc in range(8):
                        self.mm(ps[pb][:, :], mT[:, kc, tcs], wout[:, kc, hs], start=(kc == 0), stop=(kc == 7), r=mk_ + ["wout"], w=[("ps", pb)])
                    tq = t1 if half == 0 else t2
                    tqk = "t1" if half == 0 else "t2"
                    self.tt("dve", tq[:], ps[pb][:, :], self.gm_bc[:, hs], ALU.mult, r=[("ps", pb), "gm_bc"], w=[tqk])
                    self.tt("pool", xt[t][:, hs], xt[t][:, hs], tq[:], ALU.add, r=[tqk, ("dxt", t)], w=[("dxt", t)])
                self.dma(self.scr_x2[i * 128:(i + 1) * 128, :], xt[t][:], r=[("dxt", t)], w=[("scr_x2", i)])
                H2 = self.h2T[i // 8]
                col0 = (i % 8) * 128
                self.make_ht_tile(None, xt[t], ("dxt", t), xn, "dxn", 6, ("ps", 6), H2, col0, ("h2T", i), self.af, self.bfv, None)
                for kc in range(8):
                    self.mm(ps[5][:, 0:64], H2[:, kc, col0:col0 + 128], wr[:, kc, :], start=(kc == 0), stop=(kc == 7),
                            r=[("h2T", i), "wr"], w=[("ps", 5)])
                sc_, ch, tmp, mcx, sel = rt["sc"], rt["ch"], rt["tmp"], rt["mc"], rt["sel"]
                self.act(sc_[:], ps[5][:, 0:64], AF.Sigmoid, r=[("ps", 5)], w=["r_sc"])
                self.tt("dve", ch[:], sc_[:], brt[:], ALU.add, r=["r_sc", "brt"], w=["r_ch"])
                ch3 = ch[:].rearrange("p (g e) -> p g e", e=8)
                tmp3 = tmp[:].rearrange("p (g e) -> p g e", e=8)
                mc3 = mcx[:].rearrange("p (g e) -> p g e", e=8)
                self.S.op("dve", lambda e, ch3=ch3: e.tensor_reduce(out=r8["m1"][:], in_=ch3, axis=AX.X, op=ALU.max), ["r_ch"], ["r8_m1"])
                self.tt("dve", tmp3, ch3, self.bc3(r8["m1"][:], 8), ALU.is_equal, r=["r_ch", "r8_m1"], w=["r_tmp"])
                self.stt("dve", tmp[:], tmp[:], -1e9, ch[:], ALU.mult, ALU.add, r=["r_tmp", "r_ch"], w=["r_tmp"])
                self.S.op("dve", lambda e, tmp3=tmp3: e.tensor_reduce(out=r8["m2"][:], in_=tmp3, axis=AX.X, op=ALU.max), ["r_tmp"], ["r8_m2"])
                self.tt("dve", r8["gs"][:], r8["m1"][:], r8["m2"][:], ALU.add, r=["r8_m1", "r8_m2"], w=["r8_gs"])
                self.S.op("dve", lambda e: e.max(out=r8["top"][:], in_=r8["gs"][:]), ["r8_gs"], ["r8_top"])
                self.ts("dve", r8["keep"][:], r8["gs"][:], r8["top"][:, 3:4], None, ALU.is_ge, r=["r8_gs", "r8_top"], w=["r8_keep"])
                self.ts("dve", r8["pen"][:], r8["keep"][:], 1e9, -1e9, ALU.mult, ALU.add, r=["r8_keep"], w=["r8_pen"])
                self.tt("dve", mc3, ch3, self.bc3(r8["keep"][:], 8), ALU.mult, r=["r_ch", "r8_keep"], w=["r_mc"])
                self.tt("dve", mc3, mc3, self.bc3(r8["pen"][:], 8), ALU.add, r=["r_mc", "r8_pen"], w=["r_mc"])
                self.S.op("dve", lambda e: e.max(out=r8["top6"][:], in_=mcx[:]), ["r_mc"], ["r8_top6"])
                self.ts("dve", sel[:], mcx[:], r8["top6"][:, 5:6], None, ALU.is_ge, r=["r_mc", "r8_top6"], w=["r_sel"])
                self.copy("dve", self.sel_bf[:, i, :], sel[:], r=["r_sel"], w=[("sel_bf", i)])
                self.stt("dve", tmp[:], sc_[:], 1.0, sel[:], ALU.mult, ALU.mult, r=["r_sc", "r_sel", "r_tmp"], w=["r_tmp", "r8_gsum"],
                         accum_out=r8["gsum"][:, 0:1])
                self.S.op("dve", lambda e: e.reciprocal(out=r8["gsum"][:, 1:2], in_=r8["gsum"][:, 0:1]), ["r8_gsum"], ["r8_rg"])
                self.ts("dve", self.gates[:, i, :], tmp[:], r8["gsum"][:, 1:2], 2.5, ALU.mult, ALU.mult, r=["r_tmp", "r8_rg"], w=[("gates", i)])
        if self.stage >= 5:
            self.S.fence()
            M.release(mk3)
            self.moe_routing_tables()
            xt = [M.alloc("dxt%d" % i, [128, D], F32) for i in range(1)]
        if self.debug:
            d = self.dbg_out("dbg_x2", [2048, D], F32)
            d2 = self.dbg_out("dbg_h2T", [128, 8, 2048], BF16)
            self.dma(d2[:, :, 0:1024], self.h2T[0][:], r=[("h2T", i) for i in range(16)])
            self.dma(d2[:, :, 1024:2048], self.h2T[1][:], r=[("h2T", i) for i in range(16)])
            d3 = self.dbg_out("dbg_gates", [128, 1024], F32)
            self.dma(d3, self.gates[:].rearrange("p t e -> p (t e)"), r=[("gates", i) for i in range(16)])
        self.S.fence()
        if self.debug:
            d = self.dbg["dbg_x2"]
            xt0 = xt[0]
            for i in range(16):
                self.dma(xt0[:], self.scr_x2[i * 128:(i + 1) * 128, :], r=[("scr_x2", i), "dbgx"], w=["dbgx"])
                self.dma(d[i * 128:(i + 1) * 128, :], xt0[:], r=["dbgx"], w=["dbgx"])
            self.S.fence()

    def moe_routing_tables(self):
        M, I, ps, psb = self.M, self.I, self.ps, self.psb
        BIG = 1.0e6
        ones_b = M.alloc("ones_b", [128, 128], BF16)
        triu_b = M.alloc("triu_b", [128, 128], BF16)
        thr16 = M.alloc("thr16", [128, 16], F32)
        thrj = M.alloc("thrj", [128, NBLK], F32)
        iop = M.alloc("iop", [128, 1], F32)
        cnt = M.alloc("cnt", [128, 64], F32)
        nb16 = M.alloc("nb16", [128, 64, 16], F32)
        padded = M.alloc("padded", [128, 64], F32)
        pend = M.alloc("pend", [128, 64], F32)
        pstart = M.alloc("pstart", [128, 64], F32)
        accj = M.alloc("accj", [128, NBLK], F32)
        dm = M.alloc("dm", [128, 64], F32)
        d8 = M.alloc("d8", [128, 8], F32)
        oh = M.alloc("oh", [128, 64], F32)
        h2tok = [M.alloc("h2tok%d" % i, [128, D], BF16) for i in range(2)]
        self.memset("dve", ones_b[:], 1.0, w=["ones_b"])
        self.dma(triu_b[:], I["triu"], w=["triu_b"], eng="pool")
        self.dma(thr16[:], I["thr16"], w=["thr16"])
        self.dma(thrj[:], I["thrj"], w=["thrj"])
        self.dma(iop[:], I["iota_p"], w=["iop"])
        selk = [("sel_bf", i) for i in range(16)]
        for i in range(16):
            self.mm(ps[0][:, 0:64], ones_b[:], self.sel_bf[:, i, :], start=(i == 0), stop=(i == 15), r=selk + ["ones_b"], w=[("ps", 0)])
        self.copy("dve", cnt[:], ps[0][:, 0:64], r=[("ps", 0)], w=["cnt"])
        self.tt("dve", nb16[:], self.bc3(cnt[:], 16), thr16[:].unsqueeze(1).to_broadcast([128, 64, 16]), ALU.is_gt, r=["cnt", "thr16"], w=["nb16"])
        self.S.op("dve", lambda e: e.tensor_reduce(out=padded[:], in_=nb16[:], axis=AX.X, op=ALU.add), ["nb16"], ["padded0"])
        self.ts("dve", padded[:], padded[:], float(BLK), None, ALU.mult, r=["padded0"], w=["padded"])
        self.S.op("dve", lambda e: e.tensor_tensor_scan(out=pend[:], data0=padded[:], data1=padded[:], initial=0.0, op0=ALU.add, op1=ALU.max),
                  ["padded"], ["pend"])
        self.tt("dve", pstart[:], pend[:], padded[:], ALU.subtract, r=["pend", "padded"], w=["pstart"])
        self.memset("dve", accj[:], 0.0, w=["accj"])
        for e in range(N_EXP):
            self.stt("dve", accj[:], thrj[:], pend[:, e:e + 1], accj[:], ALU.is_ge, ALU.add, r=["thrj", "pend", "accj"], w=["accj"])
        self.ts("dve", accj[:], accj[:], 128.0, iop[:, 0:1], ALU.mult, ALU.add, r=["accj", "iop"], w=["accj"])
        self.copy("dve", self.widx[:], accj[:], r=["accj"], w=["widx"])
        for i in range(16):
            pb = 1 + (i % 2)
            for i2 in range(i):
                self.mm(ps[pb][:, 0:64], ones_b[:], self.sel_bf[:, i2, :], start=(i2 == 0), stop=False, r=selk + ["ones_b"], w=[("ps", pb)])
            self.mm(ps[pb][:, 0:64], triu_b[:], self.sel_bf[:, i, :], start=(i == 0), stop=True, r=selk + ["triu_b"], w=[("ps", pb)])
            self.tt("dve", dm[:], ps[pb][:, 0:64], pstart[:], ALU.add, r=[("ps", pb), "pstart"], w=["dm"])
            self.tt("dve", dm[:], dm[:], self.sel_bf[:, i, :], ALU.mult, r=["dm"] + selk, w=["dm"])
            self.ts("dve", oh[:], self.sel_bf[:, i, :], -BIG, BIG, ALU.mult, ALU.add, r=selk + ["oh"], w=["oh"])
            self.tt("dve", dm[:], dm[:], oh[:], ALU.add, r=["dm", "oh"], w=["dm"])
            self.ts("dve", oh[:], dm[:], -1.0, None, ALU.mult, r=["dm", "oh"], w=["oh"])
            self.S.op("dve", lambda e: e.max(out=d8[:], in_=oh[:]), ["oh"], ["d8"])
            self.ts("dve", d8[:], d8[:], -1.0, None, ALU.mult, r=["d8"], w=["d8"])
            self.copy("dve", self.dest6[:, i, :], d8[:], r=["d8"], w=[("dest6", i)])
            for k in range(6):
                self.ts("dve", oh[:], dm[:], d8[:, k:k + 1], None, ALU.is_equal, r=["dm", "d8", "oh"], w=["oh"])
                self.stt("dve", oh[:], oh[:], 1.0, self.gates[:, i, :], ALU.mult, ALU.mult, r=["oh", ("gates", i)], w=["oh", ("gate6", i)],
                         accum_out=self.gate6[:, i, k:k + 1])
            hb = i % 2
            H2 = self.h2T[i // 8]
            col0 = (i % 8) * 128
            for kc in range(8):
                self.tr(psb[3 + hb][:, kc * 128:(kc + 1) * 128], H2[:, kc, col0:col0 + 128], self.ident_b[:], r=[("h2T", i), "ident_b"], w=[("ps", 3 + hb)])
            self.copy("act", h2tok[hb][:], psb[3 + hb][:, 0:1024], r=[("ps", 3 + hb)], w=[("h2tok", hb)])
            for k in range(6):
                self.S.op("pool", (lambda k, i, hb: lambda e: e.indirect_dma_start(
                    out=self.xs, out_offset=bass.IndirectOffsetOnAxis(ap=self.dest6[:, i, k:k + 1], axis=0), in_=h2tok[hb][:], in_offset=None,
                    bounds_check=self.bc_reg(e, NSLOT - 1), oob_is_err=False))(k, i, hb), [("h2tok", hb), ("dest6", i)], [("xs", i, k)], dma=True)

    def phase_e(self):
        M, I, ps = self.M, self.I, self.ps
        M.release(self.e_mark1)
        yacc = M.alloc("yacc", [128, 16, D], F32)
        NW = 3
        wgu = [M.alloc("wgu%d" % i, [128, 8, 512], BF16) for i in range(NW)]
        assert M.mark() <= self.oT_end, M.mark()
        M.release(self.e_mark2)
        wdn = [M.alloc("wdn%d" % i, [128, 2, D], BF16) for i in range(NW)]
        sl = [M.alloc("sl%d" % i, [128, 512], F32) for i in range(2)]
        aT = [M.alloc("aT%d" % i, [128, 2, 512], BF16) for i in range(2)]
        gfin = M.alloc("gfin", [128, D], F32)
        xo = [M.alloc("xo%d" % i, [128, D], F32) for i in range(2)]
        tmpo = M.alloc("tmpo", [128, D], F32)
        self.dma(gfin[:], I["g_final"].partition_broadcast(128), w=["gfin"])
        NE = N_EXP + 1

        def wload(e):
            if e >= NE:
                return
            b = e % NW
            if e < N_EXP:
                g_ap = I["w_exp_gate"][e].rearrange("(kc p) n -> p kc n", p=128)
                u_ap = I["w_exp_up"][e].rearrange("(kc p) n -> p kc n", p=128)
                d_ap = I["w_exp_down"][e].rearrange("(fc p) n -> p fc n", p=128)
            else:
                g_ap = I["w_sh_gate"].rearrange("(kc p) n -> p kc n", p=128)
                u_ap = I["w_sh_up"].rearrange("(kc p) n -> p kc n", p=128)
                d_ap = I["w_sh_down"].rearrange("(fc p) n -> p fc n", p=128)
            self.dma(wgu[b][:, :, 0:256], g_ap, w=[("wgu", b, 0)], eng="pool")
            self.dma(wgu[b][:, :, 256:512], u_ap, w=[("wgu", b, 1)], eng="pool")
            self.dma(wdn[b][:], d_ap, w=[("wdn", b)], eng="pool")

        wload(0)
        wload(1)
        it = 0
        for e in range(NE):
            wload(e + 2)
            b = e % NW
            for tg in range(4):
                H2 = self.h2T[tg // 2]
                cs = slice((tg % 2) * 512, (tg % 2) * 512 + 512)
                hk = [("h2T", i) for i in range(16)] if e == 0 else []
                ab = it % 2
                for fc in range(2):
                    for kc in range(8):
                        self.mm(ps[fc][:, :], wgu[b][:, kc, fc * 128:(fc + 1) * 128], H2[:, kc, cs], start=(kc == 0), stop=(kc == 7),
                                r=hk + [("wgu", b, 0)], w=[("ps", fc)])
                    for kc in range(8):
                        self.mm(ps[2 + fc][:, :], wgu[b][:, kc, 256 + fc * 128:256 + (fc + 1) * 128], H2[:, kc, cs], start=(kc == 0), stop=(kc == 7),
                                r=hk + [("wgu", b, 1)], w=[("ps", 2 + fc)])
                for fc in range(2):
                    self.act(sl[fc][:], ps[fc][:, :], AF.Silu, r=[("ps", fc)], w=[("sl", fc)])
                    self.tt("dve", aT[ab][:, fc, :], ps[2 + fc][:, :], sl[fc][:], ALU.mult, r=[("ps", 2 + fc), ("sl", fc)], w=[("aT", ab, fc)])
                for t in range(4):
                    i = tg * 4 + t
                    for half in range(2):
                        pb = 4 + ((t * 2 + half) % 4)
                        hs = slice(half * 512, (half + 1) * 512)
                        for fc in range(2):
                            self.mm(ps[pb][:, :], aT[ab][:, fc, t * 128:(t + 1) * 128], wdn[b][:, fc, hs], start=(fc == 0), stop=(fc == 1),
                                    r=[("aT", ab, 0), ("aT", ab, 1), ("wdn", b)], w=[("ps", pb)])
                        yk = ("yacc", i, half)
                        if e == 0:
                            self.ts("dve", yacc[:, i, hs], ps[pb][:, :], self.gates[:, i, 0:1], None, ALU.mult,
                                    r=[("ps", pb), ("gates", i)], w=[yk])
                        elif e < N_EXP:
                            self.stt("dve", yacc[:, i, hs], ps[pb][:, :], self.gates[:, i, e:e + 1], yacc[:, i, hs], ALU.mult, ALU.add,
                                     r=[("ps", pb), yk], w=[yk])
                        else:
                            self.tt("dve", yacc[:, i, hs], ps[pb][:, :], yacc[:, i, hs], ALU.add, r=[("ps", pb), yk], w=[yk])
                it += 1
        for i in range(16):
            xb = i % 2
            self.dma(xo[xb][:], self.scr_x2[i * 128:(i + 1) * 128, :], r=[("scr_x2", i)], w=[("xo", xb)])
            yks = [("yacc", i, 0), ("yacc", i, 1)]
            self.tt("dve", tmpo[:], yacc[:, i, :], self.gf_bc[:], ALU.mult, r=yks + ["gf_bc"], w=["tmpo"])
            self.tt("pool", xo[xb][:], xo[xb][:], tmpo[:], ALU.add, r=["tmpo", ("xo", xb)], w=[("xo", xb)])
            c = self.stat_col()
            ssq, ms, rstd = self.stat[:, 0, c:c + 1], self.stat[:, 1, c:c + 1], self.stat[:, 2, c:c + 1]
            sk = ("st", c)
            self.stt("dve", tmpo[:], xo[xb][:], 1.0, xo[xb][:], ALU.mult, ALU.mult, r=[("xo", xb)], w=[(sk, 0), "tmpo"], accum_out=ssq)
            self.ts("dve", ms, ssq, 1.0 / D, EPS, ALU.mult, ALU.add, r=[(sk, 0)], w=[(sk, 1)])
            self.tt("pool", rstd, ms, self.mhalf[:], ALU.pow, r=[(sk, 1), "mhalf"], w=[(sk, 2)])
            self.stt("dve", xo[xb][:], xo[xb][:], rstd, gfin[:], ALU.mult, ALU.mult, r=[("xo", xb), (sk, 2), "gfin"], w=[("xo", xb)])
            self.dma(self.out[i * 128:(i + 1) * 128, :], xo[xb][:], r=[("xo", xb)], w=[("out", i)])

    def phase_e_sparse(self):
        M, I, ps, psb = self.M, self.I, self.ps, self.psb
        M.release(self.e_mark1)
        NB = 7
        NBX = 8
        wall = [M.alloc("bwall%d" % i, [128, 6144], BF16) for i in range(NB)]
        wgu = [w_[:, 0:4096].rearrange("p (k n) -> p k n", n=512) for w_ in wall]
        wd = [w_[:, 4096:6144].rearrange("p (k n) -> p k n", n=D) for w_ in wall]
        xgT = [M.alloc("xgT%d" % i, [128, 8, 128], BF16) for i in range(2)]
        sl = [M.alloc("bsl%d" % i, [128, 256], F32) for i in range(2)]
        av = [M.alloc("bav%d" % i, [128, 256], BF16) for i in range(2)]
        aT = [M.alloc("baT%d" % i, [128, 2, 128], BF16) for i in range(2)]
        yb = [M.alloc("yb%d" % i, [128, D], BF16) for i in range(2)]
        assert M.mark() <= self.oT_end, M.mark()
        M.release(self.e_mark2)
        xg = [M.alloc("xg%d" % i, [128, D], BF16) for i in range(NBX)]
        for b in range(NB):
            self.memset("dve" if b % 2 == 0 else "pool", wgu[b][:], 0.0, w=[("bwgu", b)])
            self.memset("pool" if b % 2 == 0 else "dve", wd[b][:], 0.0, w=[("bwd", b)])

        def xload_(j):
            if j >= NBLK:
                return
            bx = j % NBX
            self.dma(xg[bx][:], self.xs[j * BLK:(j + 1) * BLK, :], w=[("xg", bx)])

        def bload(j):
            if j >= NBLK:
                return
            b = j % NB
            self.S.op("pool", lambda e: e.indirect_dma_start(
                out=wall[b][:], out_offset=None, in_=self.wall_s,
                in_offset=bass.IndirectOffsetOnAxis(ap=self.widx[:, j:j + 1], axis=0), bounds_check=self.bc_reg(e, N_EXP * 128 - 1), oob_is_err=False),
                ["widx"], [("bwgu", b), ("bwd", b)], dma=True)

        def stA(j):
            b, q = j % NBX, j % 2
            for kc in range(8):
                self.tr(psb[q][:, kc * 128:(kc + 1) * 128], xg[b][:, kc * 128:(kc + 1) * 128], self.ident_b[:], r=[("xg", b), "ident_b"], w=[("ps", q)])
            self.copy("act", xgT[q][:].rearrange("p k t -> p (k t)"), psb[q][:, 0:1024], r=[("ps", q)], w=[("xgT", q)])

        def stB(j):
            b, q = j % NB, j % 2
            for kc in range(8):
                self.mm(ps[2 + q][:, :], xgT[q][:, kc, :], wgu[b][:, kc, :], start=(kc == 0), stop=(kc == 7), r=[("xgT", q), ("bwgu", b)], w=[("ps", 2 + q)])
            self.act(sl[q][:], ps[2 + q][:, 0:256], AF.Silu, r=[("ps", 2 + q)], w=[("bsl", q)])
            self.tt("dve", av[q][:], ps[2 + q][:, 256:512], sl[q][:], ALU.mult, r=[("ps", 2 + q), ("bsl", q)], w=[("bav", q)])

        def stC(j):
            q = j % 2
            for fc in range(2):
                self.tr(psb[4 + q][:, fc * 128:(fc + 1) * 128], av[q][:, fc * 128:(fc + 1) * 128], self.ident_b[:], r=[("bav", q), "ident_b"], w=[("ps", 4 + q)])
            self.copy("dve", aT[q][:].rearrange("p k t -> p (k t)"), psb[4 + q][:, 0:256], r=[("ps", 4 + q)], w=[("baT", q)])

        def stD(j):
            b, q = j % NB, j % 2
            for half in range(2):
                pb = 6 + half
                for fc in range(2):
                    self.mm(ps[pb][:, :], aT[q][:, fc, :], wd[b][:, fc, half * 512:(half + 1) * 512], start=(fc == 0), stop=(fc == 1),
                            r=[("baT", q), ("bwd", b)], w=[("ps", pb)])
                self.copy("act" if half == 0 else "dve", yb[q][:, half * 512:(half + 1) * 512], ps[pb][:, :], r=[("ps", pb)], w=[("yb", q)])
            self.dma(self.ys[j * BLK:(j + 1) * BLK, :], yb[q][:], r=[("yb", q)], w=[("ys", j)])

        for j0 in range(NB - 1):
            bload(j0)
        for j0 in range(NBX - 1):
            xload_(j0)
        stA(0)
        for j in range(NBLK):
            if j + 1 < NBLK:
                stA(j + 1)
            stB(j)
            if j >= 1:
                stD(j - 1)
            stC(j)
            bload(j + NB - 1)
            xload_(j + NBX - 1)
        stD(NBLK - 1)
        self.S.fence()
        M.release(self.e_mark1)
        wsg = M.alloc("wsg", [128, 8, 512], BF16)
        wsd = M.alloc("wsd", [128, 2, D], BF16)
        gfin = M.alloc("gfin", [128, D], F32)
        yk = [[M.alloc("yk%d_%d" % (a_, k), [128, D], BF16) for k in range(6)] for a_ in range(3)]
        acc = [M.alloc("acc%d" % i, [128, D], F32) for i in range(3)]
        xo = [M.alloc("xo%d" % i, [128, D], F32) for i in range(3)]
        dgs = [M.alloc("dg%d" % i_, [128, 6, 128], BF16) for i_ in range(3)]
        ssl = M.alloc("ssl", [128, 256], F32)
        sav = M.alloc("sav", [128, 256], BF16)
        saT = M.alloc("saT", [128, 2, 128], BF16)
        assert M.mark() <= self.oT_end, M.mark()
        self.dma(wsg[:, :, 0:256], I["w_sh_gate"].rearrange("(kc p) n -> p kc n", p=128), w=["wsg"], eng="pool")
        self.dma(wsg[:, :, 256:512], I["w_sh_up"].rearrange("(kc p) n -> p kc n", p=128), w=["wsg"], eng="pool")
        self.dma(wsd[:], I["w_sh_down"].rearrange("(fc p) n -> p fc n", p=128), w=["wsd"], eng="pool")
        self.dma(gfin[:], I["g_final"].partition_broadcast(128), w=["gfin"])

        def gload(i):
            if i >= 16:
                return
            a_ = i % 3
            for k in range(6):
                self.S.op("pool", (lambda k, a_, i: lambda e: e.indirect_dma_start(
                    out=yk[a_][k][:], out_offset=None, in_=self.ys, in_offset=bass.IndirectOffsetOnAxis(ap=self.dest6[:, i, k:k + 1], axis=0),
                    bounds_check=self.bc_reg(e, NSLOT - 1), oob_is_err=False))(k, a_, i), [], [("yk", a_, k)], dma=True)
            self.dma(xo[a_][:], self.scr_x2[i * 128:(i + 1) * 128, :], w=[("xo", a_)])

        gload(0)
        gload(1)
        for i in range(16):
            gload(i + 2)
            a_ = i % 3
            H2 = self.h2T[i // 8]
            col0 = (i % 8) * 128
            for kc in range(8):
                self.mm(ps[0][:, :], H2[:, kc, col0:col0 + 128], wsg[:, kc, :], start=(kc == 0), stop=(kc == 7), r=["wsg"], w=[("ps", 0)])
            self.act(ssl[:], ps[0][:, 0:256], AF.Silu, r=[("ps", 0)], w=["ssl"])
            self.tt("dve", sav[:], ps[0][:, 256:512], ssl[:], ALU.mult, r=[("ps", 0), "ssl"], w=["sav"])
            for fc in range(2):
                self.tr(psb[1][:, fc * 128:(fc + 1) * 128], sav[:, fc * 128:(fc + 1) * 128], self.ident_b[:], r=["sav", "ident_b"], w=[("ps", 1)])
            self.copy("dve", saT[:].rearrange("p k t -> p (k t)"), psb[1][:, 0:256], r=[("ps", 1)], w=["saT"])
            A = acc[a_]
            dg = dgs[a_]
            for k in range(6):
                self.ts("dve", dg[:, k, :], self.ident_b[:], self.gate6[:, i, k:k + 1], None, ALU.mult, r=["ident_b"], w=[("dg", a_)])
            for half in range(2):
                pb = 2 + half
                hs = slice(half * 512, (half + 1) * 512)
                for fc in range(2):
                    self.mm(ps[pb][:, :], saT[:, fc, :], wsd[:, fc, hs], start=(fc == 0), stop=False, r=["saT", "wsd"], w=[("ps", pb)])
                for k in range(6):
                    self.mm(ps[pb][:, :], dg[:, k, :], yk[a_][k][:, hs], start=False, stop=(k == 5), r=[("dg", a_), ("yk", a_, k)], w=[("ps", pb)])
                self.tt("dve", A[:, hs], ps[pb][:, :], self.gf_bc[:, hs], ALU.mult, r=[("ps", pb), "gf_bc"], w=[("acc", a_)])
            X = xo[a_]
            self.tt("dve", X[:], X[:], A[:], ALU.add, r=[("acc", a_), ("xo", a_)], w=[("xo", a_)])
            c = self.stat_col()
            ssq, ms, rstd = self.stat[:, 0, c:c + 1], self.stat[:, 1, c:c + 1], self.stat[:, 2, c:c + 1]
            sk = ("st", c)
            self.stt("dve", A[:], X[:], 1.0, X[:], ALU.mult, ALU.mult, r=[("xo", a_)], w=[(sk, 0), ("acc", a_)], accum_out=ssq)
            self.ts("dve", ms, ssq, 1.0 / D, EPS, ALU.mult, ALU.add, r=[(sk, 0)], w=[(sk, 1)])
            self.tt("pool", rstd, ms, self.mhalf[:], ALU.pow, r=[(sk, 1), "mhalf"], w=[(sk, 2)])
            self.stt("dve", X[:], X[:], rstd, gfin[:], ALU.mult, ALU.mult, r=[("xo", a_), (sk, 2), "gfin"], w=[("xo", a_)])
            self.dma(self.out[i * 128:(i + 1) * 128, :], X[:], r=[("xo", a_)], w=[("out", i)])

    def build(self):
        self.declare_inputs()
        self.precast = self.moe_precast_gen() if self.stage >= 5 else None
        self.setup_persistent()
        self.phase_a()
        self.phase_b()
        if self.stage >= 2:
            self.pass_mla()
        if self.stage >= 3:
            self.pass_fox()
        if self.stage >= 4:
            self.phase_d()
        if self.stage >= 5:
            self.phase_e_sparse()
        else:
            pass
        nc = self.nc
        sems = {e: nc.alloc_semaphore("s_" + e) for e in ENGS}
        dsems = [nc.alloc_semaphore("d%d" % i) for i in range(N_DSEM)]
        self.S.emit(sems, dsems)
        return nc


def host_inputs(inputs, core):
    b, p = core // 2, core % 2
    f32 = np.float32
    x = np.asarray(inputs["x"])[b]
    pos = np.asarray(inputs["positions"])[b].astype(np.int32)
    own = (np.arange(16)[:, None] * 2 + p) * 128 + np.arange(128)[None, :]
    own = own.reshape(-1)
    c = np.asarray(inputs["c"])[b]
    bmod = np.asarray(inputs["b_mod"])[0]

    def fm(v, n):
        return np.ascontiguousarray(np.asarray(v).reshape(n, 128).T).astype(f32)

    bmod_fm = np.concatenate([fm(bmod[s * 1024:(s + 1) * 1024], 8) for s in (0, 1, 3, 4)], axis=1)
    bmod_g = np.stack([bmod[2048:3072], bmod[5120:6144]]).astype(f32)
    bfg = np.asarray(inputs["b_forget"])[0]
    bf72 = np.zeros((128, 1), f32)
    for o in (0, 32, 64):
        bf72[o:o + 8, 0] = bfg
    invf = np.zeros((128, 1), f32)
    for r in range(128):
        invf[r, 0] = 10000.0 ** (-((r % 32) % 16) / 16.0)
    pvec = np.zeros((128, 2), f32)
    pvec[:, 0] = p
    pvec[:, 1] = 1 - p
    tri = (np.arange(128)[:, None] <= np.arange(128)[None, :]).astype(f32)
    mask0 = tri if p == 0 else np.ones((128, 128), f32)
    mask1 = np.zeros((128, 128), f32) if p == 0 else tri
    esel = np.zeros((128, 8, 128), f32)
    for h in range(8):
        for o in (0, 32, 64):
            esel[o + h, h, :] = 8.0
    sel3 = np.zeros((128, 8, 3), f32)
    for h in range(8):
        for m in range(3):
            sel3[32 * m + h, h, m] = 1.0
    d = {
        "x_all": np.ascontiguousarray(x), "x_own": np.ascontiguousarray(x[own]),
        "pos_all": np.ascontiguousarray(pos), "pos_own": np.ascontiguousarray(pos[own]),
        "c_t": fm(c, 8),
        "w_mod": np.asarray(inputs["w_mod"])[0], "bmod_fm": np.ascontiguousarray(bmod_fm), "bmod_g": bmod_g,
        "g_mix_fm": fm(np.asarray(inputs["g_mix_norm"])[0], 8), "g_ffn_fm": fm(np.asarray(inputs["g_ffn_norm"])[0], 8),
        "g_final": np.asarray(inputs["g_final"]).astype(f32),
        "w_in": np.asarray(inputs["w_in"])[0], "bf72": bf72,
        "gq_fm": fm(np.asarray(inputs["g_q_lat"])[0], 2), "gkv_fm": fm(np.asarray(inputs["g_kv_lat"])[0], 1),
        "w_q_up": np.asarray(inputs["w_q_up"])[0], "w_kv_up": np.asarray(inputs["w_kv_up"])[0],
        "w_o_mla": np.asarray(inputs["w_o_mla"])[0], "w_o_fox": np.asarray(inputs["w_o_fox"])[0], "w_out": np.asarray(inputs["w_out"])[0],
        "w_router": np.asarray(inputs["w_router"])[0], "b_router": np.asarray(inputs["b_router"])[0],
        "w_exp_gate": np.asarray(inputs["w_exp_gate"])[0], "w_exp_up": np.asarray(inputs["w_exp_up"])[0],
        "w_exp_down": np.asarray(inputs["w_exp_down"])[0],
        "w_sh_gate": np.asarray(inputs["w_sh_gate"])[0], "w_sh_up": np.asarray(inputs["w_sh_up"])[0], "w_sh_down": np.asarray(inputs["w_sh_down"])[0],
        "ident": np.eye(128, dtype=f32), "invf": invf, "pvec": pvec, "mask0": np.ascontiguousarray(mask0), "mask1": np.ascontiguousarray(mask1),
        "esel": esel.reshape(128, 1024),
        "sel3": sel3.reshape(128, 24),
        "thr16": np.broadcast_to((np.arange(16) * 128.0).astype(f32), (128, 16)),
        "thrj": np.broadcast_to((np.arange(NBLK) * float(BLK)).astype(f32), (128, NBLK)),
        "iota_p": np.arange(128, dtype=f32).reshape(128, 1),
        "triu": (np.arange(128)[:, None] < np.arange(128)[None, :]).astype(f32),
    }
    return {k: np.ascontiguousarray(v) for k, v in d.items()}, own


def kernel(**inputs):
    n = 8
    prog = Prog(stage=5, debug=False)
    nc = prog.build()
    maps, owns = [], []
    for core in range(n):
        m, own = host_inputs(inputs, core)
        maps.append(m)
        owns.append(own)
    res = run_bass_kernel_spmd(nc, maps, core_ids=list(range(n)))
    out = np.zeros((4, SEQ, D), np.float32)
    for core in range(n):
        out[core // 2, owns[core], :] = np.asarray(res.results[core]["out"], dtype=np.float32)
    return out
```

```python
import numpy as np
import ml_dtypes
import concourse.bass as bass
import concourse.mybir as mybir
from concourse.bass_utils import run_bass_kernel_spmd

F32 = mybir.dt.float32
BF16 = mybir.dt.bfloat16
I32 = mybir.dt.int32
U32 = mybir.dt.uint32
AF = mybir.ActivationFunctionType
ALU = mybir.AluOpType
AX = mybir.AxisListType

ENGS = ("pe", "act", "dve", "pool", "sp")
N_DSEM = 32
MAX_POOL_DMAS = 14


class Op:
    __slots__ = ("eng", "fn", "deps", "signal", "sig_idx", "is_dma", "dsem", "dval", "idx")

    def __init__(self, eng, fn, is_dma):
        self.eng = eng
        self.fn = fn
        self.deps = set()
        self.signal = False
        self.sig_idx = 0
        self.is_dma = is_dma
        self.dsem = -1
        self.dval = 0
        self.idx = -1


class Sched:
    def __init__(self, nc):
        self.nc = nc
        self.ops = []
        self.last_w = {}
        self.readers = {}
        self.fence_ops = None
        self.fenced = set()
        self.last_on = {e: None for e in ENGS}
        self.dma_since_fence = []
        self.n_dma = 0
        self.dsem_last = [None] * N_DSEM
        self.pool_dmas = []

    def op(self, eng, fn, reads=(), writes=(), dma=False):
        o = Op(eng, fn, dma)
        o.idx = len(self.ops)
        self.ops.append(o)
        if self.fence_ops is not None and eng not in self.fenced:
            for d in self.fence_ops:
                o.deps.add(d)
            self.fenced.add(eng)
        for k in reads:
            w = self.last_w.get(k)
            if w is not None:
                o.deps.add(w)
        for k in writes:
            w = self.last_w.get(k)
            if w is not None and (dma or self.ops[w].is_dma or self.ops[w].eng != eng):
                o.deps.add(w)
            for r in self.readers.get(k, ()):
                if dma or self.ops[r].is_dma or self.ops[r].eng != eng:
                    o.deps.add(r)
        for k in reads:
            self.readers.setdefault(k, []).append(o.idx)
        for k in writes:
            self.last_w[k] = o.idx
            self.readers[k] = []
        if dma and eng == "pool":
            self.pool_dmas.append(o.idx)
            if len(self.pool_dmas) > MAX_POOL_DMAS:
                o.deps.add(self.pool_dmas[-1 - MAX_POOL_DMAS])
        if dma:
            k = self.n_dma % N_DSEM
            self.n_dma += 1
            prev = self.dsem_last[k]
            if prev is not None:
                o.deps.add(prev)
            o.dsem = k
            o.dval = (self.ops[prev].dval if prev is not None else 0) + 16
            self.dsem_last[k] = o.idx
            self.dma_since_fence.append(o.idx)
        o.deps.discard(o.idx)
        self.last_on[eng] = o.idx
        return o.idx

    def fence(self):
        deps = [v for v in self.last_on.values() if v is not None]
        deps += self.dma_since_fence
        self.fence_ops = list(set(deps))
        self.fenced = set()
        self.dma_since_fence = []

    def emit(self, sems, dsems, final_waits=True):
        nc = self.nc
        ops = self.ops
        for o in ops:
            nd = set()
            for d in o.deps:
                do = ops[d]
                if do.is_dma:
                    nd.add(d)
                elif do.eng == o.eng and do.eng in ("pe", "sp"):
                    continue
                else:
                    do.signal = True
                    nd.add(d)
            o.deps = nd
        cnt = {e: 0 for e in ENGS}
        for o in ops:
            if not o.is_dma and o.signal:
                cnt[o.eng] += 1
                o.sig_idx = cnt[o.eng]
        per_eng = {e: [] for e in ENGS}
        for o in ops:
            per_eng[o.eng].append(o)
        engobj = {"pe": nc.tensor, "act": nc.scalar, "dve": nc.vector, "pool": nc.gpsimd, "sp": nc.sync}
        tail = [o.idx for o in ops if o.is_dma][-N_DSEM:]

        def run(ename, eobj):
            seen = {}
            for o in per_eng[ename]:
                for d in sorted(o.deps):
                    do = ops[d]
                    if do.is_dma:
                        key, val, sem = ("d", do.dsem), do.dval, dsems[do.dsem]
                    else:
                        key, val, sem = ("c", do.eng), do.sig_idx, sems[do.eng]
                    if seen.get(key, 0) >= val:
                        continue
                    seen[key] = val
                    eobj.wait_ge(sem, val)
                ins = o.fn(eobj)
                if o.is_dma:
                    ins.then_inc(dsems[o.dsem], 16)
                elif o.signal:
                    ins.then_inc(sems[o.eng], 1)
            if ename == "sp" and final_waits:
                for d in tail:
                    do = ops[d]
                    eobj.wait_ge(dsems[do.dsem], do.dval)

        with nc.Block() as block:
            @block.tensor
            def _(e):
                run("pe", e)

            @block.scalar
            def _(e):
                run("act", e)

            @block.vector
            def _(e):
                run("dve", e)

            @block.gpsimd
            def _(e):
                run("pool", e)

            @block.sync
            def _(e):
                run("sp", e)


import math

D = 1024
SEQ = 4096
NT_ALL = 32
NT_OWN = 16
EPS = 1e-6
PI = math.pi
SC_MLA = 1.0 / math.sqrt(96.0)
SC_FOX = 1.0 / 8.0
N_EXP = 64
BLK = 128
NBLK = (2048 * 6) // BLK + N_EXP
NSLOT = NBLK * BLK


def _dsize(dt):
    return mybir.dt.size(dt)


class Mem:
    def __init__(self, nc, base=16512, limit=229344):
        self.nc = nc
        self.top = base
        self.limit = limit
        self.n = 0
        self.peak = base

    def alloc(self, name, shape, dt):
        sz = int(np.prod(shape[1:])) * _dsize(dt)
        sz = (sz + 63) // 64 * 64
        t = self.nc.alloc_sbuf_tensor_at("%s_%d" % (name, self.n), list(shape), dt, offset=self.top)
        self.n += 1
        self.top += sz
        self.peak = max(self.peak, self.top)
        assert self.top <= self.limit, ("SBUF overflow", name, self.top)
        return t

    def mark(self):
        return self.top

    def release(self, m):
        self.top = m


class Prog:
    def __init__(self, stage=99, debug=False):
        self.stage = stage
        self.debug = debug
        nc = bass.Bass("TRN2", target_bir_lowering=False)
        self.nc = nc
        self.S = Sched(nc)
        self.M = Mem(nc)
        self.I = {}
        self.uid = 0
        self.ps = [nc.alloc_psum_tensor("psb%d" % i, [128, 512], F32) for i in range(8)]
        self.psb = [p[:].bitcast(BF16) for p in self.ps]

    def din(self, name, shape, dt=F32):
        ap = self.nc.dram_tensor(name, list(shape), dt, kind="ExternalInput").ap()
        self.I[name] = ap
        return ap

    def key(self, base):
        self.uid += 1
        return (base, self.uid)

    def bc_reg(self, e, val):
        if not hasattr(self, "_bc"):
            self._bc = {}
        if val not in self._bc:
            self._bc[val] = e.to_reg(val)
        return self._bc[val]

    def dma(self, out, in_, r=(), w=(), eng="sp"):
        return self.S.op(eng, lambda e: e.dma_start(out=out, in_=in_), r, w, dma=True)

    def mm(self, out, lhsT, rhs, start=True, stop=True, r=(), w=(), sgc=False):
        return self.S.op("pe", lambda e: e.matmul(out, lhsT=lhsT, rhs=rhs, start=start, stop=stop, skip_group_check=sgc), r, w)

    def tr(self, out, in_, ident, r=(), w=()):
        return self.S.op("pe", lambda e: e.transpose(out, in_, ident), r, w)

    def act(self, out, in_, func, r=(), w=(), **kw):
        return self.S.op("act", lambda e: e.activation(out=out, in_=in_, func=func, **kw), r, w)

    def ts(self, eng, out, in0, s1, s2, op0, op1=None, r=(), w=()):
        if op1 is None:
            return self.S.op(eng, lambda e: e.tensor_scalar(out=out, in0=in0, scalar1=s1, scalar2=None, op0=op0), r, w)
        return self.S.op(eng, lambda e: e.tensor_scalar(out=out, in0=in0, scalar1=s1, scalar2=s2, op0=op0, op1=op1), r, w)

    def tt(self, eng, out, in0, in1, op, r=(), w=()):
        return self.S.op(eng, lambda e: e.tensor_tensor(out=out, in0=in0, in1=in1, op=op), r, w)

    def stt(self, eng, out, in0, scalar, in1, op0, op1, r=(), w=(), accum_out=None):
        if accum_out is None:
            return self.S.op(eng, lambda e: e.scalar_tensor_tensor(out=out, in0=in0, scalar=scalar, in1=in1, op0=op0, op1=op1), r, w)
        return self.S.op(eng, lambda e: e.scalar_tensor_tensor(out=out, in0=in0, scalar=scalar, in1=in1, op0=op0, op1=op1, accum_out=accum_out), r, w)

    def copy(self, eng, out, in_, r=(), w=()):
        if eng == "act":
            return self.S.op("act", lambda e: e.copy(out=out, in_=in_), r, w)
        return self.S.op(eng, lambda e: e.tensor_copy(out=out, in_=in_), r, w)

    def memset(self, eng, ap, val, w=()):
        return self.S.op(eng, lambda e: e.memset(ap, val), (), w)

    def tss(self, eng, out, in_, scalar, op, r=(), w=()):
        return self.S.op(eng, lambda e: e.tensor_single_scalar(out=out, in_=in_, scalar=scalar, op=op), r, w)

    def declare_inputs(self):
        d = self.din
        d("x_all", [SEQ, D]); d("x_own", [2048, D])
        d("pos_all", [SEQ], I32); d("pos_own", [2048], I32)
        d("c_t", [128, 8])
        d("w_mod", [D, 6 * D]); d("bmod_fm", [128, 32]); d("bmod_g", [2, D])
        d("g_mix_fm", [128, 8]); d("g_ffn_fm", [128, 8]); d("g_final", [D])
        d("w_in", [D, 4008]); d("bf72", [128, 1]); d("gq_fm", [128, 2]); d("gkv_fm", [128, 1])
        d("w_q_up", [256, 768]); d("w_kv_up", [128, 1024])
        d("w_o_mla", [512, D]); d("w_o_fox", [512, D]); d("w_out", [D, D])
        d("w_router", [D, 64]); d("b_router", [64])
        d("w_exp_gate", [64, D, 256]); d("w_exp_up", [64, D, 256]); d("w_exp_down", [64, 256, D])
        d("w_sh_gate", [D, 256]); d("w_sh_up", [D, 256]); d("w_sh_down", [256, D])
        d("ident", [128, 128]); d("invf", [128, 1]); d("pvec", [128, 2])
        d("mask0", [128, 128]); d("mask1", [128, 128]); d("esel", [128, 1024])
        d("sel3", [128, 24]); d("thr16", [128, 16]); d("thrj", [128, NBLK]); d("iota_p", [128, 1]); d("triu", [128, 128])
        nc = self.nc
        self.scr_kf = nc.dram_tensor("scr_kf", [128, 4, SEQ], BF16, kind="Internal").ap()
        self.scr_vf = nc.dram_tensor("scr_vf", [128, 32, 520], BF16, kind="Internal").ap()
        self.scr_x2 = nc.dram_tensor("scr_x2", [2048, D], F32, kind="Internal").ap()
        self.wgu_s = nc.dram_tensor("wgu_s", [N_EXP * 128, 4096], BF16, kind="Internal").ap()
        self.wd_s = nc.dram_tensor("wd_s", [N_EXP * 128, 2048], BF16, kind="Internal").ap()
        self.xs = nc.dram_tensor("xs", [NSLOT, D], BF16, kind="Internal").ap()
        self.ys = nc.dram_tensor("ys", [NSLOT, D], BF16, kind="Internal").ap()
        self.out = nc.dram_tensor("out", [2048, D], F32, kind="ExternalOutput").ap()
        self.dbg = {}

    def dbg_out(self, name, shape, dt):
        ap = self.nc.dram_tensor(name, list(shape), dt, kind="ExternalOutput").ap()
        self.dbg[name] = ap
        return ap

    def setup_persistent(self):
        M, I = self.M, self.I
        a = M.alloc
        self.ident_f = a("ident_f", [128, 128], F32)
        self.ident_b = a("ident_b", [128, 128], BF16)
        self.m0 = a("m0", [128, 128], BF16)
        self.m1 = a("m1", [128, 128], BF16)
        self.esel = a("esel", [128, 8, 128], BF16)
        self.invf = a("invf", [128, 1], F32)
        self.pvec = a("pvec", [128, 2], F32)
        self.negbf = a("negbf", [128, 1], F32)
        self.mhalf = a("mhalf", [128, 1], F32)
        self.cond = a("cond", [128, 8], F32)
        self.modfm = a("modfm", [128, 32], F32)
        self.am = a("am", [128, 8], F32)
        self.af = a("af", [128, 8], F32)
        self.gkv = a("gkv", [128, 1], F32)
        self.gq = a("gq", [128, 2], F32)
        self.stat = a("stat", [128, 3, 256], F32)
        self.fkT = a("fkT", [128, 32, 8], F32)
        self.fkT2 = a("fkT2", [128, 32, 8], F32)
        self.pneg = a("pneg", [128, 1], F32)
        self.f3 = a("f3", [128, 2048], BF16)
        self.scn = 0
        self.dma(self.ident_f[:], I["ident"], w=["ident_f"])
        self.dma(self.ident_b[:], I["ident"], w=["ident_b"], eng="pool")
        self.dma(self.m0[:], I["mask0"], w=["m0"], eng="pool")
        self.dma(self.m1[:], I["mask1"], w=["m1"], eng="pool")
        self.dma(self.esel[:].rearrange("p h c -> p (h c)"), I["esel"], w=["esel"], eng="pool")
        self.sel3 = a("sel3", [128, 8, 3], BF16)
        self.dma(self.sel3[:].rearrange("p h c -> p (h c)"), I["sel3"], w=["sel3"], eng="pool")
        self.dma(self.invf[:], I["invf"], w=["invf"])
        self.dma(self.pvec[:], I["pvec"], w=["pvec"])
        self.dma(self.negbf[:], I["bf72"], w=["negbf0"])
        self.ts("dve", self.negbf[:], self.negbf[:], -1.0, None, ALU.mult, r=["negbf0"], w=["negbf"])
        self.memset("dve", self.mhalf[:], -0.5, w=["mhalf"])
        self.dma(self.gkv[:], I["gkv_fm"], w=["gkv"])
        self.dma(self.gq[:], I["gq_fm"], w=["gq"])
        self.memset("dve", self.f3[:], 0.0, w=["f3"])

    def stat_col(self):
        c = self.scn
        self.scn += 1
        assert c < 256
        return c

    def phase_a(self):
        M, I, ps = self.M, self.I, self.ps
        mk = M.mark()
        ct = M.alloc("ct", [128, 8], F32)
        bmod = M.alloc("bmod", [128, 32], F32)
        gmix = M.alloc("gmix", [128, 8], F32)
        gffn = M.alloc("gffn", [128, 8], F32)
        tmp8 = M.alloc("tmp8", [128, 8], F32)
        wch = [M.alloc("wch%d" % i, [128, 8, 512], BF16) for i in range(3)]
        condb = M.alloc("condb", [128, 8], BF16)
        self.dma(ct[:], I["c_t"], w=["ct"])
        self.dma(bmod[:], I["bmod_fm"], w=["bmod"])
        self.dma(gmix[:], I["g_mix_fm"], w=["gmix"])
        self.dma(gffn[:], I["g_ffn_fm"], w=["gffn"])
        self.act(self.cond[:], ct[:], AF.Silu, r=["ct"], w=["cond"])
        self.copy("dve", condb[:], self.cond[:], r=["cond"], w=["condb"])
        wm = I["w_mod"].rearrange("(kc p) n -> p kc n", p=128)
        secs = [0, 1, 3, 4]
        ci = 0
        for si, sec in enumerate(secs):
            for half in range(2):
                buf = wch[ci % 3]
                c0 = sec * 1024 + half * 512
                self.dma(buf[:], wm[:, :, c0:c0 + 512], w=[("wch", ci % 3)], eng="pool")
                for jb in range(4):
                    col = si * 8 + half * 4 + jb
                    for kc in range(8):
                        self.mm(ps[0][:, col:col + 1], buf[:, kc, jb * 128:(jb + 1) * 128], condb[:, kc:kc + 1],
                                start=(kc == 0), stop=(kc == 7), r=[("wch", ci % 3), "condb"], w=["psA"])
                ci += 1
        self.tt("dve", self.modfm[:], ps[0][:, 0:32], bmod[:], ALU.add, r=["psA", "bmod"], w=["modfm"])
        self.ts("dve", tmp8[:], self.modfm[:, 8:16], 1.0, None, ALU.add, r=["modfm"], w=["tmp8"])
        self.tt("dve", self.am[:], tmp8[:], gmix[:], ALU.mult, r=["tmp8", "gmix"], w=["am"])
        self.ts("dve", tmp8[:], self.modfm[:, 24:32], 1.0, None, ALU.add, r=["modfm"], w=["tmp8"])
        self.tt("dve", self.af[:], tmp8[:], gffn[:], ALU.mult, r=["tmp8", "gffn"], w=["af"])
        self.bm = self.modfm[:, 0:8]
        self.bfv = self.modfm[:, 16:24]
        self.S.fence()
        M.release(mk)

    def ht_fe1(self, x_rows, xt, xt_key, xn, xn_key, xn_eng="act"):
        c = self.stat_col()
        ssq = self.stat[:, 0, c:c + 1]
        ms = self.stat[:, 1, c:c + 1]
        rstd = self.stat[:, 2, c:c + 1]
        sk = ("st", c)
        if x_rows is not None:
            self.dma(xt[:], x_rows, w=[xt_key])
        self.stt("dve", xn[:], xt[:], 1.0, xt[:], ALU.mult, ALU.mult, r=[xt_key], w=[(sk, 0), xn_key], accum_out=ssq)
        self.ts("dve", ms, ssq, 1.0 / D, EPS, ALU.mult, ALU.add, r=[(sk, 0)], w=[(sk, 1)])
        self.tt("pool", rstd, ms, self.mhalf[:], ALU.pow, r=[(sk, 1), "mhalf"], w=[(sk, 2)])
        if xn_eng == "act":
            self.act(xn[:], xt[:], AF.Copy, r=[xt_key, (sk, 2)], w=[xn_key], scale=rstd)
        else:
            self.ts(xn_eng, xn[:], xt[:], rstd, None, ALU.mult, r=[xt_key, (sk, 2)], w=[xn_key])

    def ht_fe2(self, xn, xn_key, psbank, ps_key, hT, col0, hkey, a_sc, b_sc):
        pb = self.psb[psbank]
        for kc in range(8):
            self.tr(pb[:, kc * 128:(kc + 1) * 128], xn[:, kc * 128:(kc + 1) * 128], self.ident_b[:],
                    r=[xn_key, "ident_b"], w=[ps_key])
        for kc in range(8):
            self.ts("dve", hT[:, kc, col0:col0 + 128], pb[:, kc * 128:(kc + 1) * 128], a_sc[:, kc:kc + 1], b_sc[:, kc:kc + 1],
                    ALU.mult, ALU.add, r=[ps_key, "am", "af", "modfm"], w=[hkey])

    def make_ht_tile(self, x_rows, xt, xt_key, xn, xn_key, psbank, ps_key, hT, col0, hkey, a_sc, b_sc, junk, xn_eng="act"):
        self.ht_fe1(x_rows, xt, xt_key, xn, xn_key, xn_eng)
        self.ht_fe2(xn, xn_key, psbank, ps_key, hT, col0, hkey, a_sc, b_sc)

    def rope_tables(self, pos_ap, bufs):
        R = slice(64, 96)
        cosT, sinT, tA, tB = bufs["cosT"], bufs["sinT"], bufs["tA"], bufs["tB"]
        posi = tB[:].bitcast(I32)
        kc_, ks, ka, kb = [(bufs["k"], n) for n in ("cosT", "sinT", "tA", "tB")]
        kp = kb
        self.dma(posi[:, :], pos_ap.partition_broadcast(128), w=[kp])
        self.ts("dve", tA[R, :], posi[R, :], self.invf[R, 0:1], None, ALU.mult, r=[kp, "invf"], w=[ka])
        self.ts("dve", posi[R, :], tA[R, :], 1.0 / (2 * PI), None, ALU.mult, r=[ka], w=[kp])
        self.copy("dve", tB[R, :], posi[R, :], r=[kp], w=[kb])
        self.stt("dve", tA[R, :], tB[R, :], -2 * PI, tA[R, :], ALU.mult, ALU.add, r=[kb, ka], w=[ka])

        def wrap(dst, shift, dk):
            self.ts("dve", dst[R, :], tA[R, :], shift, None, ALU.add, r=[ka], w=[dk])
            self.tss("dve", tB[R, :], dst[R, :], PI, ALU.is_gt, r=[dk], w=[kb])
            self.stt("dve", dst[R, :], tB[R, :], -2 * PI, dst[R, :], ALU.mult, ALU.add, r=[kb, dk], w=[dk])
            self.tss("dve", tB[R, :], dst[R, :], -PI, ALU.is_lt, r=[dk], w=[kb])
            self.stt("dve", dst[R, :], tB[R, :], 2 * PI, dst[R, :], ALU.mult, ALU.add, r=[kb, dk], w=[dk])
            self.act(dst[R, :], dst[R, :], AF.Sin, r=[dk], w=[dk])

        wrap(sinT, 0.0, ks)
        wrap(cosT, PI / 2, kc_)
        return kc_, ks

    def phase_b(self):
        M, I, ps, psb = self.M, self.I, self.ps, self.psb
        self.kv_mark = M.mark()
        self.ktm = M.alloc("ktm", [128, 8, SEQ], BF16)
        self.vm = M.alloc("vm", [128, 32, 8, 65], BF16)
        self.oT_off = M.mark()
        self.oT = M.alloc("oT", [128, 8, 2048], BF16)
        self.oT_end = M.mark()
        M.release(self.oT_off)
        mk = M.mark()
        win = I["w_in"].rearrange("(kc p) n -> p kc n", p=128)
        wk_tok = M.alloc("wk_tok", [128, 8, 640], BF16)
        wk_fk = M.alloc("wk_fk", [128, 8, 512], BF16)
        wk_r = M.alloc("wk_r", [128, 8, 32], BF16)
        wk_rot = M.alloc("wk_rot", [128, 8, 32], BF16)
        wf3 = M.alloc("wf3", [128, 8, 72], BF16)
        wkv_k = M.alloc("wkv_k", [128, 8, 64], BF16)
        wkv_v = M.alloc("wkv_v", [128, 8, 64], BF16)
        self.dma(wk_tok[:, :, 0:128], win[:, :, 256:384], w=["wk_tok"], eng="pool")
        self.dma(wk_tok[:, :, 128:640], win[:, :, 1440:1952], w=["wk_tok"], eng="pool")
        self.dma(wk_fk[:], win[:, :, 928:1440], w=["wk_fk"], eng="pool")
        self.dma(wk_r[:], win[:, :, 384:416], w=["wk_r"], eng="pool")
        self.dma(wk_rot[:, :, 0:16], win[:, :, 400:416], w=["wk_rot"], eng="pool")
        self.dma(wk_rot[:, :, 16:32], win[:, :, 384:400], w=["wk_rot"], eng="pool")
        self.ts("dve", wk_rot[:, :, 0:16], wk_rot[:, :, 0:16], -1.0, None, ALU.mult, r=["wk_rot"], w=["wk_rot"])
        self.memset("dve", wf3[:], 0.0, w=["wf3"])
        for o3 in (0, 32, 64):
            self.dma(wf3[:, :, o3:o3 + 8], win[:, :, 1952:1960], w=["wf3"], eng="pool")
        wkv = I["w_kv_up"].rearrange("f (h c) -> f h c", c=128)
        self.dma(wkv_k[:], wkv[:, :, 0:64], w=["wkv_k"], eng="pool")
        self.dma(wkv_v[:], wkv[:, :, 64:128], w=["wkv_v"], eng="pool")
        self.memset("pool", self.vm[:, :, :, 64:65], 1.0, w=["vm_ones"])

        NX = 4
        xt = [M.alloc("xt%d" % i, [128, D], F32) for i in range(NX)]
        xn = [M.alloc("xn%d" % i, [128, D], BF16) for i in range(2)]
        hT = [M.alloc("hT%d" % i, [128, 8, 512], BF16) for i in range(2)]
        kvl = [M.alloc("kvl%d" % i, [128, 128], F32) for i in range(2)]
        kvn = [M.alloc("kvn%d" % i, [128, 128], BF16) for i in range(2)]
        kvnT = [M.alloc("kvnT%d" % i, [128, 512], BF16) for i in range(2)]
        kfs = M.alloc("kfs", [128, 4, 512], BF16)
        vfs = [M.alloc("vfs%d" % i_, [128, 4, 8, 65], BF16) for i_ in range(2)]
        fT = M.alloc("fT", [128, SEQ], F32)
        rb = {"cosT": M.alloc("cosT", [128, 512], F32),
              "sinT": M.alloc("sinT", [128, 512], F32), "tA": M.alloc("tA", [128, 512], F32),
              "tB": M.alloc("tB", [128, 512], F32), "k": "rbB"}
        for v_ in vfs:
            self.memset("pool", v_[:, :, :, 64:65], 1.0, w=["vfs"])
        self.memset("dve", fT[:], 0.0, w=["fT"])
        fm_i = 0

        def xload(i):
            if i < 32:
                self.dma(xt[i % NX][:], I["x_all"][i * 128:(i + 1) * 128, :], w=[("xt", i % NX)])

        def fe1(i):
            if i < 32:
                self.ht_fe1(None, xt[i % NX], ("xt", i % NX), xn[i % 2], ("xn", i % 2))

        def fe2(i):
            fe2_pe(i)
            fe2_dve(i)

        def fe2_pe(i):
            if i < 32:
                pb = self.psb[i % 2]
                for kc in range(8):
                    self.tr(pb[:, kc * 128:(kc + 1) * 128], xn[i % 2][:, kc * 128:(kc + 1) * 128], self.ident_b[:],
                            r=[("xn", i % 2), "ident_b"], w=[("ps", i % 2)])

        def fe2_dve(i):
            if i < 32:
                pb = self.psb[i % 2]
                H_ = hT[(i // 4) % 2]
                col0 = (i % 4) * 128
                for kc in range(8):
                    self.ts("dve", H_[:, kc, col0:col0 + 128], pb[:, kc * 128:(kc + 1) * 128], self.am[:, kc:kc + 1], self.bm[:, kc:kc + 1],
                            ALU.mult, ALU.add, r=[("ps", i % 2), "am", "modfm"], w=[("hT", (i // 4) % 2, i % 4)])

        st = {"fm_i": 0, "rope": {}}

        def be1_pe(i):
            g, t = i // 4, i % 4
            hb = g % 2
            H = hT[hb]
            tcs = slice(t * 128, (t + 1) * 128)
            hk = [("hT", hb, t)]
            pa = 2 + (i % 2)
            for kc in range(8):
                self.mm(ps[pa][:, :], H[:, kc, tcs], wk_tok[:, kc, 128:640], start=(kc == 0), stop=(kc == 7),
                        r=hk + ["wk_tok"], w=[("ps", pa)])
            for kc in range(8):
                self.mm(ps[4][:, t * 128:(t + 1) * 128], H[:, kc, tcs], wk_tok[:, kc, 0:128], start=(kc == 0), stop=(kc == 7),
                        r=hk + ["wk_tok"], w=[("ps4", t)])

        def be1_rest(i):
            g, t = i // 4, i % 4
            pa = 2 + (i % 2)
            self.copy("act", vfs[g % 2][:, t, :, 0:64], ps[pa][:, :].rearrange("p (h c) -> p h c", c=64), r=[("ps", pa)], w=["vfs"])
            c = self.stat_col()
            st["kvc"] = c
            ssq, ms, rstd = self.stat[:, 0, c:c + 1], self.stat[:, 1, c:c + 1], self.stat[:, 2, c:c + 1]
            sk = ("st", c)
            kb = i % 2
            self.copy("dve", kvl[kb][:], ps[4][:, t * 128:(t + 1) * 128], r=[("ps4", t)], w=[("kvl", kb)])
            self.stt("dve", kvn[kb][:], kvl[kb][:], 1.0, kvl[kb][:], ALU.mult, ALU.mult, r=[("kvl", kb)], w=[(sk, 0), ("kvn", kb)], accum_out=ssq)
            self.ts("dve", ms, ssq, 1.0 / 128, EPS, ALU.mult, ALU.add, r=[(sk, 0)], w=[(sk, 1)])
            self.tt("pool", rstd, ms, self.mhalf[:], ALU.pow, r=[(sk, 1), "mhalf"], w=[(sk, 2)])

        def be1_tail(i):
            c = st["kvc"]
            rstd = self.stat[:, 2, c:c + 1]
            sk = ("st", c)
            kb = i % 2
            self.ts("dve", kvn[kb][:], kvl[kb][:], rstd, None, ALU.mult, r=[("kvl", kb), (sk, 2)], w=[("kvn", kb)])

        def be2(i):
            g, t = i // 4, i % 4
            hb = g % 2
            tcs = slice(t * 128, (t + 1) * 128)
            kb = i % 2
            pa = 2 + (i % 2)
            self.tr(psb[5][:, t * 128:(t + 1) * 128], kvn[kb][:], self.ident_b[:], r=[("kvn", kb), "ident_b"], w=[("ps5", t)])
            self.ts("dve", kvnT[hb][:, tcs], psb[5][:, t * 128:(t + 1) * 128], self.gkv[:, 0:1], None, ALU.mult,
                    r=[("ps5", t), "gkv"], w=[("kvnT", hb, t)])
            self.mm(ps[pa][:, :], kvnT[hb][:, tcs], wkv_v[:].rearrange("p h c -> p (h c)"), r=[("kvnT", hb, t), "wkv_v"], w=[("ps", pa)])
            self.copy("act", self.vm[:, i, :, 0:64], ps[pa][:, :].rearrange("p (h c) -> p h c", c=64), r=[("ps", pa)], w=[("vm", i)])

        def grp(g):
            hb = g % 2
            H = hT[hb]
            gc = slice(g * 512, (g + 1) * 512)
            kcos, ksin = st["rope"][g]
            hk = [("hT", hb, t) for t in range(4)]
            kvk = [("kvnT", hb, t) for t in range(4)]
            for pr in range(4):
                pf = 6 + (st["fm_i"] % 2); st["fm_i"] += 1
                for kc in range(8):
                    self.mm(ps[pf][:, :], wk_fk[:, kc, pr * 128:(pr + 1) * 128], H[:, kc, :], start=(kc == 0), stop=(kc == 7),
                            r=hk + ["wk_fk"], w=[("ps", pf)])
                self.copy("act" if pr % 2 == 0 else "dve", kfs[:, pr, :], ps[pf][:, :], r=[("ps", pf)], w=["kfs"])
            for h in range(8):
                pf = 6 + (st["fm_i"] % 2); st["fm_i"] += 1
                self.mm(ps[pf][0:64, :], wkv_k[:, h, :], kvnT[hb][:, :], r=kvk + ["wkv_k"], w=[("ps", pf)])
                self.copy("act" if h % 2 == 0 else "dve", self.ktm[0:64, h, gc], ps[pf][0:64, :], r=[("ps", pf)], w=[("ktm", g)])
            for kc in range(8):
                self.mm(ps[6][64:96, :], wk_r[:, kc, :], H[:, kc, :], start=(kc == 0), stop=(kc == 7), r=hk + ["wk_r"], w=[("ps", 6)])
            for kc in range(8):
                self.mm(ps[7][64:96, :], wk_rot[:, kc, :], H[:, kc, :], start=(kc == 0), stop=(kc == 7), r=hk + ["wk_rot"], w=[("ps", 7)])
            st["fm_i"] += 2
            R = slice(64, 96)
            self.tt("dve", rb["tA"][R, :], ps[6][R, :], rb["cosT"][R, :], ALU.mult, r=[("ps", 6), kcos], w=[("rbB", "tA")])
            self.tt("dve", rb["tB"][R, :], ps[7][R, :], rb["sinT"][R, :], ALU.mult, r=[("ps", 7), ksin], w=[("rbB", "tB")])
            self.tt("dve", self.ktm[R, 0, gc], rb["tA"][R, :], rb["tB"][R, :], ALU.add, r=[("rbB", "tA"), ("rbB", "tB")], w=[("ktm", g)])
            for h in range(1, 8):
                self.copy("act" if h % 2 == 1 else "pool", self.ktm[R, h, gc], self.ktm[R, 0, gc], r=[("ktm", g)], w=[("ktmr", g, h)])
            pf = 6 + (st["fm_i"] % 2); st["fm_i"] += 1
            for kc in range(8):
                self.mm(ps[pf][0:72, :], wf3[:, kc, :], H[:, kc, :], start=(kc == 0), stop=(kc == 7), r=hk + ["wf3"], w=[("ps", pf)])
            self.act(fT[0:72, gc], ps[pf][0:72, :], AF.Exp, r=[("ps", pf), "negbf"], w=[("fT", g)], scale=-1.0, bias=self.negbf[0:72, 0:1])
            self.act(fT[0:72, gc], fT[0:72, gc], AF.Ln, r=[("fT", g)], w=[("fT", g)], bias=1.0)
            self.dma(self.scr_kf[:, :, gc], kfs[:], r=["kfs"], w=[("scr_kf", g)])
            self.dma(self.scr_vf[:, 4 * g:4 * g + 4, :], vfs[g % 2][:].rearrange("p t h c -> p t (h c)"), r=["vfs"], w=[("scr_vf", g)])

        xload(0)
        xload(1)
        xload(2)
        fe1(0)
        fe1(1)
        fe2(0)
        for i in range(32):
            g, t = i // 4, i % 4
            xload(i + 3)
            fe2_pe(i + 1)
            be1_pe(i)
            fe2_dve(i + 1)
            be1_rest(i)
            fe1(i + 2)
            be1_tail(i)
            if t == 1:
                st["rope"][g] = self.rope_tables(I["pos_all"][g * 512:(g + 1) * 512], rb)
            if i >= 1:
                be2(i - 1)
                if (i - 1) % 4 == 3:
                    grp((i - 1) // 4)
        be2(31)
        grp(7)
        fkeys = [("fT", g) for g in range(8)]
        self.S.op("dve", lambda e: e.tensor_tensor_scan(out=fT[0:72, :], data0=fT[0:72, :], data1=fT[0:72, :], initial=0.0,
                                                         op0=ALU.add, op1=ALU.max), fkeys, ["fTs"])
        for i in range(32):
            self.tr(ps[0][:, i * 8:(i + 1) * 8], fT[0:8, i * 128:(i + 1) * 128], self.ident_f[0:8, 0:8], r=["fTs", "ident_f"], w=[("ps", 0)])
        self.copy("dve", self.fkT[:].rearrange("p t h -> p (t h)"), ps[0][:, 0:256], r=[("ps", 0)], w=["fkT"])
        self.ts("dve", self.pneg[:], self.pvec[:, 1:2], -1e30, None, ALU.mult, r=["pvec"], w=["pneg"])
        self.ts("dve", self.fkT2[:].rearrange("p t h -> p (t h)"), self.fkT[:].rearrange("p t h -> p (t h)"), self.pneg[:, 0:1], None,
                ALU.add, r=["fkT", "pneg"], w=["fkT2"])
        fv4 = fT[0:72, :].rearrange("p (j two r) -> p j two r", two=2, r=128)
        for hf in range(2):
            js = slice(hf * 8, hf * 8 + 8)
            ev = fv4[:, js, 0, :]
            od = fv4[:, js, 1, :]
            T0 = xt[0][0:72, :].rearrange("p (j r) -> p j r", r=128)
            T1 = xt[1][0:72, :].rearrange("p (j r) -> p j r", r=128)
            self.ts("dve", T0, ev, self.pvec[0:72, 1:2], None, ALU.mult, r=["fTs", "pvec", ("xt", 0)], w=[("xt", 0)])
            self.stt("dve", T0, od, self.pvec[0:72, 0:1], T0, ALU.mult, ALU.add, r=["fTs", "pvec", ("xt", 0)], w=[("xt", 0)])
            self.ts("dve", xt[0][0:72, :], xt[0][0:72, :], -1.0, None, ALU.mult, r=[("xt", 0)], w=[("xt", 0)])
            hi = xn[0]; mid = xn[1]
            cs = slice(hf * 1024, (hf + 1) * 1024)
            self.copy("dve", hi[0:72, :], xt[0][0:72, :], r=[("xt", 0)], w=[("xn", 0)])
            self.tt("dve", xt[1][0:72, :], xt[0][0:72, :], hi[0:72, :], ALU.subtract, r=[("xt", 0), ("xn", 0)], w=[("xt", 1)])
            self.copy("dve", mid[0:72, :], xt[1][0:72, :], r=[("xt", 1)], w=[("xn", 1)])
            self.tt("dve", xt[2][0:72, :], xt[1][0:72, :], mid[0:72, :], ALU.subtract, r=[("xt", 1), ("xn", 1)], w=[("xt", 2)])
            self.copy("dve", self.f3[0:8, cs], hi[0:8, :], r=[("xn", 0), "f3"], w=["f3"])
            self.copy("dve", self.f3[32:40, cs], mid[32:40, :], r=[("xn", 1), "f3"], w=["f3"])
            self.copy("dve", self.f3[64:72, cs], xt[2][64:72, :], r=[("xt", 2), "f3"], w=["f3"])
        if self.debug:
            d1 = self.dbg_out("dbg_ktm", [128, 8, SEQ], BF16)
            self.dma(d1, self.ktm[:], r=[("ktm", g) for g in range(8)] + [("ktmr", g, h) for g in range(8) for h in range(1, 8)])
            d2 = self.dbg_out("dbg_vm", [128, 32, 520], BF16)
            self.dma(d2, self.vm[:].rearrange("p t h c -> p t (h c)"), r=[("vm", i) for i in range(32)] + ["vm_ones"])
            d3 = self.dbg_out("dbg_fkT", [128, 256], F32)
            self.dma(d3, self.fkT[:].rearrange("p t h -> p (t h)"), r=["fkT"])
            d4 = self.dbg_out("dbg_f3", [128, 2048], BF16)
            self.dma(d4, self.f3[:], r=["f3"])
        self.S.fence()
        M.release(mk)

    def qside_common_alloc(self):
        M = self.M
        self.q_xt = [M.alloc("qxt%d" % i, [128, D], F32) for i in range(1)]
        self.q_xn = M.alloc("qxn", [128, D], BF16)
        self.q_hT = M.alloc("qhT", [128, 8, 512], BF16)
        self.pbuf = [M.alloc("pbuf%d" % i, [128, 512], BF16) for i in range(4)]
        self.otok = M.alloc("otok", [128, 4, 512], BF16)
        self.rc = M.alloc("rc", [128, 8], F32)

    def qside_gen(self, g, fox, W):
        I, ps, psb = self.I, self.ps, self.psb
        H = self.q_hT
        qb = g % 2
        for t in range(4):
            i = 4 * g + t
            xs = 0
            self.ht_fe1(I["x_own"][i * 128:(i + 1) * 128, :], self.q_xt[xs], ("qxt", xs), self.q_xn, "qxn", xn_eng="dve")
            yield
            yield
            self.ht_fe2(self.q_xn, "qxn", 5, ("ps", 5), H, t * 128, ("qhT", t), self.am, self.bm)
            yield
            yield
            if not fox:
                tcs = slice(t * 128, (t + 1) * 128)
                for kc in range(8):
                    self.mm(ps[6][:, 0:256], H[:, kc, tcs], W["w_ql"][:, kc, :], start=(kc == 0), stop=(kc == 7),
                            r=[("qhT", t), "w_ql"], w=[("ps6", "a")])
                yield
                c = self.stat_col()
                ssq, ms, rstd = self.stat[:, 0, c:c + 1], self.stat[:, 1, c:c + 1], self.stat[:, 2, c:c + 1]
                sk = ("st", c)
                self.copy("dve", W["ql"][:], ps[6][:, 0:256], r=[("ps6", "a")], w=["ql"])
                self.stt("dve", W["qn"][:], W["ql"][:], 1.0, W["ql"][:], ALU.mult, ALU.mult, r=["ql"], w=[(sk, 0), "qn"], accum_out=ssq)
                self.ts("dve", ms, ssq, 1.0 / 256, EPS, ALU.mult, ALU.add, r=[(sk, 0)], w=[(sk, 1)])
                self.tt("pool", rstd, ms, self.mhalf[:], ALU.pow, r=[(sk, 1), "mhalf"], w=[(sk, 2)])
                self.ts("dve", W["qn"][:], W["ql"][:], rstd, None, ALU.mult, r=["ql", (sk, 2)], w=["qn"])
                yield
                yield
                for kc in range(2):
                    self.tr(psb[6][:, 512 + kc * 128:512 + (kc + 1) * 128], W["qn"][:, kc * 128:(kc + 1) * 128], self.ident_b[:],
                            r=["qn", "ident_b"], w=[("ps6", "b")])
                for kc in range(2):
                    self.ts("dve", W["qnT"][:, kc, tcs], psb[6][:, 512 + kc * 128:512 + (kc + 1) * 128], self.gq[:, kc:kc + 1], None,
                            ALU.mult, r=[("ps6", "b"), "gq"], w=[("qnT", t)])
                yield
        hk = [("qhT", t) for t in range(4)]
        if fox:
            QT = W["qtf"][qb]
            for h in range(8):
                for kc in range(8):
                    self.mm(ps[7][0:64, :], W["w_fq"][:, kc, h * 64:(h + 1) * 64], H[:, kc, :], start=(kc == 0), stop=(kc == 7),
                            r=hk + ["w_fq"], w=[("ps", 7)])
                self.mm(ps[6][64:67, :], self.sel3[0:72, h, :], self.f3[0:72, g * 512:(g + 1) * 512], r=["sel3", "f3"], w=[("ps", 6)])
                yield
                self.copy("dve", QT[0:64, h, :], ps[7][0:64, :], r=[("ps", 7)], w=[("qtf", qb)])
                self.copy("dve", QT[64:67, h, :], ps[6][64:67, :], r=[("ps", 6)], w=[("qtf", qb)])
                yield
        else:
            QT = W["qtm"][qb]
            rbq = W["rbq"]
            kcos, ksin = self.rope_tables(I["pos_own"][g * 512:(g + 1) * 512], rbq)
            yield
            qk = [("qnT", t) for t in range(4)]
            R = slice(64, 96)
            for h in range(8):
                for kc in range(2):
                    self.mm(ps[7][0:96, :], W["wq_up"][:, kc, h * 96:(h + 1) * 96], W["qnT"][:, kc, :], start=(kc == 0), stop=(kc == 1),
                            r=qk + ["wq_up"], w=[("ps", 7)])
                for kc in range(2):
                    self.mm(ps[5][64:96, 0:512], W["wq_rot"][:, kc, h, :], W["qnT"][:, kc, :], start=(kc == 0), stop=(kc == 1),
                            r=qk + ["wq_rot"], w=[("ps", 5)])
                yield
                self.copy("dve", QT[0:64, h, :], ps[7][0:64, :], r=[("ps", 7)], w=[("qtm", qb)])
                self.tt("dve", rbq["tA"][R, :], ps[7][R, :], rbq["cosT"][R, :], ALU.mult, r=[("ps", 7), kcos], w=[("rbq", "tA")])
                self.tt("dve", rbq["tB"][R, :], ps[5][R, 0:512], rbq["sinT"][R, :], ALU.mult, r=[("ps", 5), ksin], w=[("rbq", "tB")])
                self.tt("dve", QT[R, h, :], rbq["tA"][R, :], rbq["tB"][R, :], ALU.add, r=[("rbq", "tA"), ("rbq", "tB")], w=[("qtm", qb)])
                yield

    def attn_group(self, g, fox, W, side_gen, extra_gen=None):
        ps, psb = self.ps, self.psb
        qb = g % 2
        nkb = 8 * g + 8
        tiles = []
        for h in range(8):
            for n in range(nkb):
                if n < 8 * g:
                    tiles.append((h, n, 0, None))
                else:
                    m = n - 8 * g
                    tiles.append((h, n, (m // 2) * 128, self.m0 if m % 2 == 0 else self.m1))
        NT = len(tiles)
        base = 4 if fox else 0
        sc = SC_FOX if fox else SC_MLA

        def qk(i):
            h, n, c0, _ = tiles[i]
            bk = i % 3
            ncs = slice(n * 128, (n + 1) * 128)
            if fox:
                self.mm(ps[bk][:, c0:512], W["ktf"][0:67, h, ncs], W["qtf"][qb][0:67, h, c0:512], start=True, stop=True,
                        r=[("qtf", qb), "ktf"], w=[("ps", bk)])
            else:
                self.mm(ps[bk][:, c0:512], self.ktm[0:96, h, ncs], W["qtm"][qb][0:96, h, c0:512], start=True, stop=True,
                        r=[("qtm", qb)], w=[("ps", bk)])

        def expo(i):
            h, n, c0, mk = tiles[i]
            bk = i % 3
            pb = i % 4
            P_ = self.pbuf[pb]
            if fox and mk is self.m1:
                self.act(P_[:, c0:c0 + 128], ps[bk][:, c0:c0 + 128], AF.Exp, r=[("ps", bk), "fkT2"], w=[("pbuf", pb)], scale=sc,
                         bias=self.fkT2[:, n, h:h + 1])
                if c0 + 128 < 512:
                    self.act(P_[:, c0 + 128:512], ps[bk][:, c0 + 128:512], AF.Exp, r=[("ps", bk), "fkT"], w=[("pbuf", pb)], scale=sc,
                             bias=self.fkT[:, n, h:h + 1])
            elif fox:
                self.act(P_[:, c0:512], ps[bk][:, c0:512], AF.Exp, r=[("ps", bk), "fkT"], w=[("pbuf", pb)], scale=sc,
                         bias=self.fkT[:, n, h:h + 1])
            else:
                self.act(P_[:, c0:512], ps[bk][:, c0:512], AF.Exp, r=[("ps", bk)], w=[("pbuf", pb)], scale=sc)
            if mk is not None:
                self.tt("pool", P_[:, c0:c0 + 128], P_[:, c0:c0 + 128], mk[:], ALU.mult, r=[("pbuf", pb), "m0", "m1"], w=[("pbuf", pb)])

        def pv(i):
            h, n, c0, _ = tiles[i]
            pb = i % 4
            ob = 3 + (h % 2)
            P_ = self.pbuf[pb]
            V = W["vf"] if fox else self.vm
            for jj in range(c0 // 128, 4):
                first = (n == 0 and jj == 0)
                last = (n == nkb - 1 and jj == 3)
                self.mm(ps[ob][:, jj * 65:(jj + 1) * 65], P_[:, jj * 128:(jj + 1) * 128], V[:, n, h, :], start=first, stop=last,
                        r=[("pbuf", pb), "vf"], w=[("ps", ob)], sgc=True)
            if n == nkb - 1:
                o4 = ps[ob][:, 0:260].rearrange("p (j c) -> p j c", c=65)
                self.S.op("dve", lambda e: e.reciprocal(out=self.rc[:, 0:4], in_=o4[:, :, 64]), [("ps", ob)], ["rc"])
                for jj in range(4):
                    self.ts("dve", self.otok[:, jj, h * 64:(h + 1) * 64], ps[ob][:, jj * 65:jj * 65 + 64], self.rc[:, jj:jj + 1], None,
                            ALU.mult, r=[("ps", ob), "rc"], w=[("otok", h)])

        side_every = max(1, NT // (48 if fox else 64))
        qk(0)
        if NT > 1:
            qk(1)
        for i in range(NT):
            expo(i)
            if i + 2 < NT:
                qk(i + 2)
            pv(i)
            if i % side_every == side_every - 1:
                if side_gen is not None:
                    next(side_gen, None)
            if extra_gen is not None and i % 6 == 5:
                next(extra_gen, None)
        if side_gen is not None:
            for _ in side_gen:
                pass
        for rnd in range(2):
            for jl in range(2):
                jj = rnd * 2 + jl
                for c in range(4):
                    self.tr(psb[5][:, (jl * 4 + c) * 128:(jl * 4 + c + 1) * 128], self.otok[:, jj, c * 128:(c + 1) * 128], self.ident_b[:],
                            r=[("otok", hh) for hh in range(8)] + ["ident_b"], w=[("ps", 5)])
            for jl in range(2):
                jj = rnd * 2 + jl
                t0 = g * 512 + jj * 128
                self.copy("dve", self.oT[:, base:base + 4, t0:t0 + 128],
                          psb[5][:, jl * 512:(jl + 1) * 512].rearrange("p (c t) -> p c t", t=128), r=[("ps", 5)], w=[("oT", base, g)])

    def moe_precast_gen(self):
        I = self.I
        for e in range(N_EXP):
            rows = slice(e * 128, (e + 1) * 128)
            gu = self.wgu_s[rows, :].rearrange("p (kc n) -> p kc n", n=512)
            self.dma(gu[:, :, 0:256], I["w_exp_gate"][e].rearrange("(kc p) n -> p kc n", p=128), w=[("wgu_s", e, 0)], eng="pool")
            yield
            self.dma(gu[:, :, 256:512], I["w_exp_up"][e].rearrange("(kc p) n -> p kc n", p=128), w=[("wgu_s", e, 1)], eng="pool")
            yield
            self.dma(self.wd_s[rows, :].rearrange("p (fc n) -> p fc n", n=D), I["w_exp_down"][e].rearrange("(fc p) n -> p fc n", p=128),
                     w=[("wd_s", e)], eng="pool")
            yield

    def pass_mla(self):
        M, I = self.M, self.I
        M.release(self.oT_end)
        mk = M.mark()
        self.qside_common_alloc()
        W = {}
        W["w_ql"] = M.alloc("w_ql", [128, 8, 256], BF16)
        W["wq_up"] = M.alloc("wq_up", [128, 2, 768], BF16)
        W["wq_rot"] = M.alloc("wq_rot", [128, 2, 8, 32], BF16)
        W["ql"] = M.alloc("ql", [128, 256], F32)
        W["qn"] = M.alloc("qn", [128, 256], BF16)
        W["qnT"] = M.alloc("qnT", [128, 2, 512], BF16)
        W["qtm"] = [M.alloc("qtm%d" % i, [128, 8, 512], BF16) for i in range(2)]
        W["rbq"] = {"cosT": M.alloc("qcosT", [128, 512], F32),
                    "sinT": M.alloc("qsinT", [128, 512], F32), "tA": M.alloc("qtA", [128, 512], F32),
                    "tB": M.alloc("qtB", [128, 512], F32), "k": "rbq"}
        win = I["w_in"].rearrange("(kc p) n -> p kc n", p=128)
        self.dma(W["w_ql"][:], win[:, :, 0:256], w=["w_ql"], eng="pool")
        wq = I["w_q_up"].rearrange("(kc p) n -> p kc n", p=128)
        self.dma(W["wq_up"][:], wq, w=["wq_up"], eng="pool")
        wq4 = I["w_q_up"].rearrange("(kc p) (h c) -> p kc h c", p=128, c=96)
        for kc in range(2):
            self.dma(W["wq_rot"][:, kc, :, 0:16], wq4[:, kc, :, 80:96], w=["wq_rot"], eng="pool")
            self.dma(W["wq_rot"][:, kc, :, 16:32], wq4[:, kc, :, 64:80], w=["wq_rot"], eng="pool")
        self.ts("dve", W["wq_rot"][:, :, :, 0:16], W["wq_rot"][:, :, :, 0:16], -1.0, None, ALU.mult, r=["wq_rot"], w=["wq_rot"])
        for _ in self.qside_gen(0, False, W):
            pass
        for g in range(4):
            gen = self.qside_gen(g + 1, False, W) if g < 3 else None
            self.attn_group(g, False, W, gen, self.precast)
        if self.debug:
            d = self.dbg_out("dbg_oT_mla", [128, 4, 2048], BF16)
            self.dma(d, self.oT[:, 0:4, :], r=[("oT", 0, g) for g in range(4)])
        self.S.fence()
        M.release(mk)

    def pass_fox(self):
        M, I = self.M, self.I
        M.release(self.kv_mark)
        self.attn_alloc_common_after = None
        W = {}
        W["ktf"] = M.alloc("ktf", [128, 8, SEQ], BF16)
        W["vf"] = M.alloc("vf", [128, 32, 8, 65], BF16)
        assert M.mark() <= self.oT_off
        M.release(self.oT_end)
        mk = M.mark()
        self.qside_common_alloc()
        W["w_fq"] = M.alloc("w_fq", [128, 8, 512], BF16)
        W["qtf"] = [M.alloc("qtf%d" % i, [128, 8, 512], BF16) for i in range(2)]
        win = I["w_in"].rearrange("(kc p) n -> p kc n", p=128)
        self.dma(W["w_fq"][:], win[:, :, 416:928], w=["w_fq"], eng="pool")
        for pr in range(4):
            for hf in range(2):
                self.dma(W["ktf"][0:64, 2 * pr + hf, :], self.scr_kf[hf * 64:(hf + 1) * 64, pr, :], r=[("scr_kf", gg) for gg in range(8)], w=["ktf"])
        self.memset("dve", W["ktf"][64:67, :, :], 8.0, w=["ktf"])
        for q in range(4):
            ts_ = slice(q * 8, (q + 1) * 8)
            self.dma(W["vf"][:, ts_, :, :].rearrange("p t h c -> p t (h c)"), self.scr_vf[:, ts_, :],
                     r=[("scr_vf", gg) for gg in range(8)], w=["vf"])
        for _ in self.qside_gen(0, True, W):
            pass
        for g in range(4):
            gen = self.qside_gen(g + 1, True, W) if g < 3 else None
            self.attn_group(g, True, W, gen, self.precast)
        if self.precast is not None:
            for _ in self.precast:
                pass
        if self.debug:
            d = self.dbg_out("dbg_oT_fox", [128, 4, 2048], BF16)
            self.dma(d, self.oT[:, 4:8, :], r=[("oT", 4, g) for g in range(4)])
        self.S.fence()
        M.release(mk)
        self.post_mark = self.kv_mark

    def bc3(self, ap2, n):
        return ap2.unsqueeze(2).to_broadcast([128, ap2.shape[1], n])

    def phase_d(self):
        M, I, ps, psb = self.M, self.I, self.ps, self.psb
        M.release(self.kv_mark)
        self.gm_bc = M.alloc("gm_bc", [128, D], F32)
        self.gf_bc = M.alloc("gf_bc", [128, D], F32)
        self.h2T = [M.alloc("h2T_a", [128, 8, 1024], BF16), None]
        wr = M.alloc("wr", [128, 8, 64], BF16)
        brt = M.alloc("brt", [128, 64], F32)
        self.sel_bf = M.alloc("sel_bf", [128, 16, 64], BF16)
        self.dest6 = M.alloc("dest6", [128, 16, 8], U32)
        self.gate6 = M.alloc("gate6", [128, 16, 8], F32)
        self.widx = M.alloc("widx", [128, NBLK], U32)
        self.e_mark1 = M.mark()
        wg = M.alloc("wg", [128, 8, 2048], BF16)
        wo_m = M.alloc("wo_m", [128, 4, D], BF16)
        wo_f = M.alloc("wo_f", [128, 4, D], BF16)
        wout = M.alloc("wout", [128, 8, D], BF16)
        assert M.mark() <= self.oT_off, M.mark()
        M.release(self.oT_end)
        self.h2T[1] = M.alloc("h2T_b", [128, 8, 1024], BF16)
        self.e_mark2 = M.mark()
        mk2 = M.mark()
        cbc = M.alloc("cbc", [128, 8, 128], F32)
        ones = M.alloc("ones", [128, 128], F32)
        wch = [M.alloc("wchd%d" % i, [128, 8, 512], F32) for i in range(2)]
        self.memset("dve", ones[:], 1.0, w=["ones"])
        for kc in range(8):
            self.ts("dve", cbc[:, kc, :], ones[:], self.cond[:, kc:kc + 1], None, ALU.mult, r=["ones", "cond"], w=["cbc"])
        self.dma(self.gm_bc[:], I["bmod_g"][0].partition_broadcast(128), w=["gm_bc"])
        self.dma(self.gf_bc[:], I["bmod_g"][1].partition_broadcast(128), w=["gf_bc"])
        wm = I["w_mod"].rearrange("(kc p) n -> p kc n", p=128)
        ci = 0
        for sec, dst, dk in ((2, self.gm_bc, "gm_bc"), (5, self.gf_bc, "gf_bc")):
            for half in range(2):
                buf = wch[ci % 2]
                c0 = sec * 1024 + half * 512
                self.dma(buf[:], wm[:, :, c0:c0 + 512], w=[("wchd", ci % 2)])
                pb = ci % 2
                for kc in range(8):
                    self.mm(ps[pb][:, :], cbc[:, kc, :], buf[:, kc, :], start=(kc == 0), stop=(kc == 7),
                            r=["cbc", ("wchd", ci % 2)], w=[("ps", pb)])
                self.tt("dve", dst[:, half * 512:(half + 1) * 512], ps[pb][:, :], dst[:, half * 512:(half + 1) * 512], ALU.add,
                        r=[("ps", pb), dk], w=[dk])
                ci += 1
        win = I["w_in"].rearrange("(kc p) n -> p kc n", p=128)
        self.dma(wg[:, :, 0:1024], win[:, :, 1960:2984], w=["wg"], eng="pool")
        self.dma(wg[:, :, 1024:2048], win[:, :, 2984:4008], w=["wg"], eng="pool")
        self.dma(wo_m[:], I["w_o_mla"].rearrange("(c p) n -> p c n", p=128), w=["wo_m"], eng="pool")
        self.dma(wo_f[:], I["w_o_fox"].rearrange("(c p) n -> p c n", p=128), w=["wo_f"], eng="pool")
        self.dma(wout[:], I["w_out"].rearrange("(kc p) n -> p kc n", p=128), w=["wout"], eng="pool")
        self.dma(wr[:], I["w_router"].rearrange("(kc p) n -> p kc n", p=128), w=["wr"], eng="pool")
        self.dma(brt[:], I["b_router"].partition_broadcast(128), w=["brt"])
        self.S.fence()
        M.release(mk2)
        rt = {n: M.alloc("r_" + n, [128, 64], F32) for n in ("sc", "ch", "tmp", "mc", "sel")}
        self.gates = M.alloc("gates", [128, 16, 64], F32)
        r8 = {n: M.alloc("r8_" + n, [128, 8], F32) for n in ("m1", "m2", "gs", "top", "keep", "pen", "top6", "gsum")}
        mk3 = M.mark()
        xt = [M.alloc("dxt%d" % i, [128, D], F32) for i in range(4)]
        xn = M.alloc("dxn", [128, D], BF16)
        hT = M.alloc("dhT", [128, 8, 512], BF16)
        sa = M.alloc("sa", [128, 512], F32)
        sb_ = M.alloc("sb", [128, 512], F32)
        t1 = M.alloc("t1", [128, 512], F32)
        t2 = M.alloc("t2", [128, 512], F32)
        mT = M.alloc("mT", [128, 8, 512], BF16)
        for g in range(4):
            gcs = slice(g * 512, (g + 1) * 512)
            for t in range(4):
                i = 4 * g + t
                self.make_ht_tile(I["x_own"][i * 128:(i + 1) * 128, :], xt[t], ("dxt", t), xn, "dxn",
                                  6, ("ps", 6), hT, t * 128, ("dhT", t), self.am, self.bm, None)
            hk = [("dhT", t) for t in range(4)]
            ok = [("oT", 0, g), ("oT", 4, g)]
            for mc in range(8):
                b0 = 0 if mc % 2 == 0 else 2
                ms_ = slice(mc * 128, (mc + 1) * 128)
                for kc in range(8):
                    self.mm(ps[b0][:, :], wg[:, kc, ms_], hT[:, kc, :], start=(kc == 0), stop=(kc == 7), r=hk + ["wg"], w=[("ps", b0)])
                for kc in range(8):
                    self.mm(ps[b0 + 1][:, :], wg[:, kc, 1024 + mc * 128:1024 + (mc + 1) * 128], hT[:, kc, :], start=(kc == 0), stop=(kc == 7),
                            r=hk + ["wg"], w=[("ps", b0 + 1)])
                for c in range(4):
                    self.mm(ps[4][:, :], wo_m[:, c, ms_], self.oT[:, c, gcs], start=(c == 0), stop=(c == 3), r=ok + ["wo_m"], w=[("ps", 4)])
                for c in range(4):
                    self.mm(ps[5][:, :], wo_f[:, c, ms_], self.oT[:, 4 + c, gcs], start=(c == 0), stop=(c == 3), r=ok + ["wo_f"], w=[("ps", 5)])
                self.act(sa[:], ps[b0][:, :], AF.Sigmoid, r=[("ps", b0)], w=["sa"])
                self.act(sb_[:], ps[b0 + 1][:, :], AF.Sigmoid, r=[("ps", b0 + 1)], w=["sb"])
                self.tt("dve", t1[:], ps[4][:, :], sa[:], ALU.mult, r=[("ps", 4), "sa"], w=["t1"])
                self.tt("dve", t2[:], ps[5][:, :], sb_[:], ALU.mult, r=[("ps", 5), "sb"], w=["t2"])
                self.tt("pool", mT[:, mc, :], t1[:], t2[:], ALU.add, r=["t1", "t2"], w=[("mT", mc)])
            mk_ = [("mT", mc) for mc in range(8)]
            for t in range(4):
                i = 4 * g + t
                tcs = slice(t * 128, (t + 1) * 128)
                for half in range(2):
                    pb = 6 + half
                    hs = slice(half * 512, (half + 1) * 512)
                    for kc in range(8):
                        self.mm(ps[pb][:, :], mT[:, kc, tcs], wout[:, kc, hs], start=(kc == 0), stop=(kc == 7), r=mk_ + ["wout"], w=[("ps", pb)])
                    tq = t1 if half == 0 else t2
                    tqk = "t1" if half == 0 else "t2"
                    self.tt("dve", tq[:], ps[pb][:, :], self.gm_bc[:, hs], ALU.mult, r=[("ps", pb), "gm_bc"], w=[tqk])
                    self.tt("pool", xt[t][:, hs], xt[t][:, hs], tq[:], ALU.add, r=[tqk, ("dxt", t)], w=[("dxt", t)])
                self.dma(self.scr_x2[i * 128:(i + 1) * 128, :], xt[t][:], r=[("dxt", t)], w=[("scr_x2", i)])
                H2 = self.h2T[i // 8]
                col0 = (i % 8) * 128
                self.make_ht_tile(None, xt[t], ("dxt", t), xn, "dxn", 6, ("ps", 6), H2, col0, ("h2T", i), self.af, self.bfv, None)
                for kc in range(8):
                    self.mm(ps[5][:, 0:64], H2[:, kc, col0:col0 + 128], wr[:, kc, :], start=(kc == 0), stop=(kc == 7),
                            r=[("h2T", i), "wr"], w=[("ps", 5)])
                sc_, ch, tmp, mcx, sel = rt["sc"], rt["ch"], rt["tmp"], rt["mc"], rt["sel"]
                self.act(sc_[:], ps[5][:, 0:64], AF.Sigmoid, r=[("ps", 5)], w=["r_sc"])
                self.tt("dve", ch[:], sc_[:], brt[:], ALU.add, r=["r_sc", "brt"], w=["r_ch"])
                ch3 = ch[:].rearrange("p (g e) -> p g e", e=8)
                tmp3 = tmp[:].rearrange("p (g e) -> p g e", e=8)
                mc3 = mcx[:].rearrange("p (g e) -> p g e", e=8)
                self.S.op("dve", lambda e, ch3=ch3: e.tensor_reduce(out=r8["m1"][:], in_=ch3, axis=AX.X, op=ALU.max), ["r_ch"], ["r8_m1"])
                self.tt("dve", tmp3, ch3, self.bc3(r8["m1"][:], 8), ALU.is_equal, r=["r_ch", "r8_m1"], w=["r_tmp"])
                self.stt("dve", tmp[:], tmp[:], -1e9, ch[:], ALU.mult, ALU.add, r=["r_tmp", "r_ch"], w=["r_tmp"])
                self.S.op("dve", lambda e, tmp3=tmp3: e.tensor_reduce(out=r8["m2"][:], in_=tmp3, axis=AX.X, op=ALU.max), ["r_tmp"], ["r8_m2"])
                self.tt("dve", r8["gs"][:], r8["m1"][:], r8["m2"][:], ALU.add, r=["r8_m1", "r8_m2"], w=["r8_gs"])
                self.S.op("dve", lambda e: e.max(out=r8["top"][:], in_=r8["gs"][:]), ["r8_gs"], ["r8_top"])
                self.ts("dve", r8["keep"][:], r8["gs"][:], r8["top"][:, 3:4], None, ALU.is_ge, r=["r8_gs", "r8_top"], w=["r8_keep"])
                self.ts("dve", r8["pen"][:], r8["keep"][:], 1e9, -1e9, ALU.mult, ALU.add, r=["r8_keep"], w=["r8_pen"])
                self.tt("dve", mc3, ch3, self.bc3(r8["keep"][:], 8), ALU.mult, r=["r_ch", "r8_keep"], w=["r_mc"])
                self.tt("dve", mc3, mc3, self.bc3(r8["pen"][:], 8), ALU.add, r=["r_mc", "r8_pen"], w=["r_mc"])
                self.S.op("dve", lambda e: e.max(out=r8["top6"][:], in_=mcx[:]), ["r_mc"], ["r8_top6"])
                self.ts("dve", sel[:], mcx[:], r8["top6"][:, 5:6], None, ALU.is_ge, r=["r_mc", "r8_top6"], w=["r_sel"])
                self.copy("dve", self.sel_bf[:, i, :], sel[:], r=["r_sel"], w=[("sel_bf", i)])
                self.stt("dve", tmp[:], sc_[:], 1.0, sel[:], ALU.mult, ALU.mult, r=["r_sc", "r_sel", "r_tmp"], w=["r_tmp", "r8_gsum"],
                         accum_out=r8["gsum"][:, 0:1])
                self.S.op("dve", lambda e: e.reciprocal(out=r8["gsum"][:, 1:2], in_=r8["gsum"][:, 0:1]), ["r8_gsum"], ["r8_rg"])
                self.ts("dve", self.gates[:, i, :], tmp[:], r8["gsum"][:, 1:2], 2.5, ALU.mult, ALU.mult, r=["r_tmp", "r8_rg"], w=[("gates", i)])
        if self.stage >= 5:
            self.S.fence()
            M.release(mk3)
            self.moe_routing_tables()
            xt = [M.alloc("dxt%d" % i, [128, D], F32) for i in range(1)]
        if self.debug:
            d = self.dbg_out("dbg_x2", [2048, D], F32)
            d2 = self.dbg_out("dbg_h2T", [128, 8, 2048], BF16)
            self.dma(d2[:, :, 0:1024], self.h2T[0][:], r=[("h2T", i) for i in range(16)])
            self.dma(d2[:, :, 1024:2048], self.h2T[1][:], r=[("h2T", i) for i in range(16)])
            d3 = self.dbg_out("dbg_gates", [128, 1024], F32)
            self.dma(d3, self.gates[:].rearrange("p t e -> p (t e)"), r=[("gates", i) for i in range(16)])
        self.S.fence()
        if self.debug:
            d = self.dbg["dbg_x2"]
            xt0 = xt[0]
            for i in range(16):
                self.dma(xt0[:], self.scr_x2[i * 128:(i + 1) * 128, :], r=[("scr_x2", i), "dbgx"], w=["dbgx"])
                self.dma(d[i * 128:(i + 1) * 128, :], xt0[:], r=["dbgx"], w=["dbgx"])
            self.S.fence()

    def moe_routing_tables(self):
        M, I, ps, psb = self.M, self.I, self.ps, self.psb
        BIG = 1.0e6
        ones_b = M.alloc("ones_b", [128, 128], BF16)
        triu_b = M.alloc("triu_b", [128, 128], BF16)
        thr16 = M.alloc("thr16", [128, 16], F32)
        thrj = M.alloc("thrj", [128, NBLK], F32)
        iop = M.alloc("iop", [128, 1], F32)
        cnt = M.alloc("cnt", [128, 64], F32)
        nb16 = M.alloc("nb16", [128, 64, 16], F32)
        padded = M.alloc("padded", [128, 64], F32)
        pend = M.alloc("pend", [128, 64], F32)
        pstart = M.alloc("pstart", [128, 64], F32)
        accj = M.alloc("accj", [128, NBLK], F32)
        dm = M.alloc("dm", [128, 64], F32)
        d8 = M.alloc("d8", [128, 8], F32)
        oh = M.alloc("oh", [128, 64], F32)
        h2tok = [M.alloc("h2tok%d" % i, [128, D], BF16) for i in range(2)]
        self.memset("dve", ones_b[:], 1.0, w=["ones_b"])
        self.dma(triu_b[:], I["triu"], w=["triu_b"], eng="pool")
        self.dma(thr16[:], I["thr16"], w=["thr16"])
        self.dma(thrj[:], I["thrj"], w=["thrj"])
        self.dma(iop[:], I["iota_p"], w=["iop"])
        selk = [("sel_bf", i) for i in range(16)]
        for i in range(16):
            self.mm(ps[0][:, 0:64], ones_b[:], self.sel_bf[:, i, :], start=(i == 0), stop=(i == 15), r=selk + ["ones_b"], w=[("ps", 0)])
        self.copy("dve", cnt[:], ps[0][:, 0:64], r=[("ps", 0)], w=["cnt"])
        self.tt("dve", nb16[:], self.bc3(cnt[:], 16), thr16[:].unsqueeze(1).to_broadcast([128, 64, 16]), ALU.is_gt, r=["cnt", "thr16"], w=["nb16"])
        self.S.op("dve", lambda e: e.tensor_reduce(out=padded[:], in_=nb16[:], axis=AX.X, op=ALU.add), ["nb16"], ["padded0"])
        self.ts("dve", padded[:], padded[:], float(BLK), None, ALU.mult, r=["padded0"], w=["padded"])
        self.S.op("dve", lambda e: e.tensor_tensor_scan(out=pend[:], data0=padded[:], data1=padded[:], initial=0.0, op0=ALU.add, op1=ALU.max),
                  ["padded"], ["pend"])
        self.tt("dve", pstart[:], pend[:], padded[:], ALU.subtract, r=["pend", "padded"], w=["pstart"])
        self.memset("dve", accj[:], 0.0, w=["accj"])
        for e in range(N_EXP):
            self.stt("dve", accj[:], thrj[:], pend[:, e:e + 1], accj[:], ALU.is_ge, ALU.add, r=["thrj", "pend", "accj"], w=["accj"])
        self.ts("dve", accj[:], accj[:], 128.0, iop[:, 0:1], ALU.mult, ALU.add, r=["accj", "iop"], w=["accj"])
        self.copy("dve", self.widx[:], accj[:], r=["accj"], w=["widx"])
        for i in range(16):
            pb = 1 + (i % 2)
            for i2 in range(i):
                self.mm(ps[pb][:, 0:64], ones_b[:], self.sel_bf[:, i2, :], start=(i2 == 0), stop=False, r=selk + ["ones_b"], w=[("ps", pb)])
            self.mm(ps[pb][:, 0:64], triu_b[:], self.sel_bf[:, i, :], start=(i == 0), stop=True, r=selk + ["triu_b"], w=[("ps", pb)])
            self.tt("dve", dm[:], ps[pb][:, 0:64], pstart[:], ALU.add, r=[("ps", pb), "pstart"], w=["dm"])
            self.tt("dve", dm[:], dm[:], self.sel_bf[:, i, :], ALU.mult, r=["dm"] + selk, w=["dm"])
            self.ts("dve", oh[:], self.sel_bf[:, i, :], -BIG, BIG, ALU.mult, ALU.add, r=selk + ["oh"], w=["oh"])
            self.tt("dve", dm[:], dm[:], oh[:], ALU.add, r=["dm", "oh"], w=["dm"])
            self.ts("dve", oh[:], dm[:], -1.0, None, ALU.mult, r=["dm", "oh"], w=["oh"])
            self.S.op("dve", lambda e: e.max(out=d8[:], in_=oh[:]), ["oh"], ["d8"])
            self.ts("dve", d8[:], d8[:], -1.0, None, ALU.mult, r=["d8"], w=["d8"])
            self.copy("dve", self.dest6[:, i, :], d8[:], r=["d8"], w=[("dest6", i)])
            for k in range(6):
                self.ts("dve", oh[:], dm[:], d8[:, k:k + 1], None, ALU.is_equal, r=["dm", "d8", "oh"], w=["oh"])
                self.stt("dve", oh[:], oh[:], 1.0, self.gates[:, i, :], ALU.mult, ALU.mult, r=["oh", ("gates", i)], w=["oh", ("gate6", i)],
                         accum_out=self.gate6[:, i, k:k + 1])
            hb = i % 2
            H2 = self.h2T[i // 8]
            col0 = (i % 8) * 128
            for kc in range(8):
                self.tr(psb[3 + hb][:, kc * 128:(kc + 1) * 128], H2[:, kc, col0:col0 + 128], self.ident_b[:], r=[("h2T", i), "ident_b"], w=[("ps", 3 + hb)])
            self.copy("act", h2tok[hb][:], psb[3 + hb][:, 0:1024], r=[("ps", 3 + hb)], w=[("h2tok", hb)])
            for k in range(6):
                self.S.op("pool", (lambda k, i, hb: lambda e: e.indirect_dma_start(
                    out=self.xs, out_offset=bass.IndirectOffsetOnAxis(ap=self.dest6[:, i, k:k + 1], axis=0), in_=h2tok[hb][:], in_offset=None,
                    bounds_check=self.bc_reg(e, NSLOT - 1), oob_is_err=False))(k, i, hb), [("h2tok", hb), ("dest6", i)], [("xs", i, k)], dma=True)

    def phase_e(self):
        M, I, ps = self.M, self.I, self.ps
        M.release(self.e_mark1)
        yacc = M.alloc("yacc", [128, 16, D], F32)
        NW = 3
        wgu = [M.alloc("wgu%d" % i, [128, 8, 512], BF16) for i in range(NW)]
        assert M.mark() <= self.oT_end, M.mark()
        M.release(self.e_mark2)
        wdn = [M.alloc("wdn%d" % i, [128, 2, D], BF16) for i in range(NW)]
        sl = [M.alloc("sl%d" % i, [128, 512], F32) for i in range(2)]
        aT = [M.alloc("aT%d" % i, [128, 2, 512], BF16) for i in range(2)]
        gfin = M.alloc("gfin", [128, D], F32)
        xo = [M.alloc("xo%d" % i, [128, D], F32) for i in range(2)]
        tmpo = M.alloc("tmpo", [128, D], F32)
        self.dma(gfin[:], I["g_final"].partition_broadcast(128), w=["gfin"])
        NE = N_EXP + 1

        def wload(e):
            if e >= NE:
                return
            b = e % NW
            if e < N_EXP:
                g_ap = I["w_exp_gate"][e].rearrange("(kc p) n -> p kc n", p=128)
                u_ap = I["w_exp_up"][e].rearrange("(kc p) n -> p kc n", p=128)
                d_ap = I["w_exp_down"][e].rearrange("(fc p) n -> p fc n", p=128)
            else:
                g_ap = I["w_sh_gate"].rearrange("(kc p) n -> p kc n", p=128)
                u_ap = I["w_sh_up"].rearrange("(kc p) n -> p kc n", p=128)
                d_ap = I["w_sh_down"].rearrange("(fc p) n -> p fc n", p=128)
            self.dma(wgu[b][:, :, 0:256], g_ap, w=[("wgu", b, 0)], eng="pool")
            self.dma(wgu[b][:, :, 256:512], u_ap, w=[("wgu", b, 1)], eng="pool")
            self.dma(wdn[b][:], d_ap, w=[("wdn", b)], eng="pool")

        wload(0)
        wload(1)
        it = 0
        for e in range(NE):
            wload(e + 2)
            b = e % NW
            for tg in range(4):
                H2 = self.h2T[tg // 2]
                cs = slice((tg % 2) * 512, (tg % 2) * 512 + 512)
                hk = [("h2T", i) for i in range(16)] if e == 0 else []
                ab = it % 2
                for fc in range(2):
                    for kc in range(8):
                        self.mm(ps[fc][:, :], wgu[b][:, kc, fc * 128:(fc + 1) * 128], H2[:, kc, cs], start=(kc == 0), stop=(kc == 7),
                                r=hk + [("wgu", b, 0)], w=[("ps", fc)])
                    for kc in range(8):
                        self.mm(ps[2 + fc][:, :], wgu[b][:, kc, 256 + fc * 128:256 + (fc + 1) * 128], H2[:, kc, cs], start=(kc == 0), stop=(kc == 7),
                                r=hk + [("wgu", b, 1)], w=[("ps", 2 + fc)])
                for fc in range(2):
                    self.act(sl[fc][:], ps[fc][:, :], AF.Silu, r=[("ps", fc)], w=[("sl", fc)])
                    self.tt("dve", aT[ab][:, fc, :], ps[2 + fc][:, :], sl[fc][:], ALU.mult, r=[("ps", 2 + fc), ("sl", fc)], w=[("aT", ab, fc)])
                for t in range(4):
                    i = tg * 4 + t
                    for half in range(2):
                        pb = 4 + ((t * 2 + half) % 4)
                        hs = slice(half * 512, (half + 1) * 512)
                        for fc in range(2):
                            self.mm(ps[pb][:, :], aT[ab][:, fc, t * 128:(t + 1) * 128], wdn[b][:, fc, hs], start=(fc == 0), stop=(fc == 1),
                                    r=[("aT", ab, 0), ("aT", ab, 1), ("wdn", b)], w=[("ps", pb)])
                        yk = ("yacc", i, half)
                        if e == 0:
                            self.ts("dve", yacc[:, i, hs], ps[pb][:, :], self.gates[:, i, 0:1], None, ALU.mult,
                                    r=[("ps", pb), ("gates", i)], w=[yk])
                        elif e < N_EXP:
                            self.stt("dve", yacc[:, i, hs], ps[pb][:, :], self.gates[:, i, e:e + 1], yacc[:, i, hs], ALU.mult, ALU.add,
                                     r=[("ps", pb), yk], w=[yk])
                        else:
                            self.tt("dve", yacc[:, i, hs], ps[pb][:, :], yacc[:, i, hs], ALU.add, r=[("ps", pb), yk], w=[yk])
                it += 1
        for i in range(16):
            xb = i % 2
            self.dma(xo[xb][:], self.scr_x2[i * 128:(i + 1) * 128, :], r=[("scr_x2", i)], w=[("xo", xb)])
            yks = [("yacc", i, 0), ("yacc", i, 1)]
            self.tt("dve", tmpo[:], yacc[:, i, :], self.gf_bc[:], ALU.mult, r=yks + ["gf_bc"], w=["tmpo"])
            self.tt("pool", xo[xb][:], xo[xb][:], tmpo[:], ALU.add, r=["tmpo", ("xo", xb)], w=[("xo", xb)])
            c = self.stat_col()
            ssq, ms, rstd = self.stat[:, 0, c:c + 1], self.stat[:, 1, c:c + 1], self.stat[:, 2, c:c + 1]
            sk = ("st", c)
            self.stt("dve", tmpo[:], xo[xb][:], 1.0, xo[xb][:], ALU.mult, ALU.mult, r=[("xo", xb)], w=[(sk, 0), "tmpo"], accum_out=ssq)
            self.ts("dve", ms, ssq, 1.0 / D, EPS, ALU.mult, ALU.add, r=[(sk, 0)], w=[(sk, 1)])
            self.tt("pool", rstd, ms, self.mhalf[:], ALU.pow, r=[(sk, 1), "mhalf"], w=[(sk, 2)])
            self.stt("dve", xo[xb][:], xo[xb][:], rstd, gfin[:], ALU.mult, ALU.mult, r=[("xo", xb), (sk, 2), "gfin"], w=[("xo", xb)])
            self.dma(self.out[i * 128:(i + 1) * 128, :], xo[xb][:], r=[("xo", xb)], w=[("out", i)])

    def phase_e_sparse(self):
        M, I, ps, psb = self.M, self.I, self.ps, self.psb
        M.release(self.e_mark1)
        NB = 7
        NBX = 8
        wgu = [M.alloc("bwgu%d" % i, [128, 8, 512], BF16) for i in range(NB)]
        wd = [M.alloc("bwd%d" % i, [128, 2, D], BF16) for i in range(NB)]
        xgT = [M.alloc("xgT%d" % i, [128, 8, 128], BF16) for i in range(2)]
        sl = [M.alloc("bsl%d" % i, [128, 256], F32) for i in range(2)]
        av = [M.alloc("bav%d" % i, [128, 256], BF16) for i in range(2)]
        aT = [M.alloc("baT%d" % i, [128, 2, 128], BF16) for i in range(2)]
        yb = [M.alloc("yb%d" % i, [128, D], BF16) for i in range(2)]
        assert M.mark() <= self.oT_end, M.mark()
        M.release(self.e_mark2)
        xg = [M.alloc("xg%d" % i, [128, D], BF16) for i in range(NBX)]
        for b in range(NB):
            self.memset("dve" if b % 2 == 0 else "pool", wgu[b][:], 0.0, w=[("bwgu", b)])
            self.memset("pool" if b % 2 == 0 else "dve", wd[b][:], 0.0, w=[("bwd", b)])

        def xload_(j):
            if j >= NBLK:
                return
            bx = j % NBX
            self.dma(xg[bx][:], self.xs[j * BLK:(j + 1) * BLK, :], w=[("xg", bx)])

        def bload(j):
            if j >= NBLK:
                return
            b = j % NB
            self.S.op("pool", lambda e: e.indirect_dma_start(
                out=wgu[b][:].rearrange("p k n -> p (k n)"), out_offset=None, in_=self.wgu_s,
                in_offset=bass.IndirectOffsetOnAxis(ap=self.widx[:, j:j + 1], axis=0), bounds_check=self.bc_reg(e, N_EXP * 128 - 1), oob_is_err=False),
                ["widx"], [("bwgu", b)], dma=True)
            self.S.op("pool", lambda e: e.indirect_dma_start(
                out=wd[b][:].rearrange("p k n -> p (k n)"), out_offset=None, in_=self.wd_s,
                in_offset=bass.IndirectOffsetOnAxis(ap=self.widx[:, j:j + 1], axis=0), bounds_check=self.bc_reg(e, N_EXP * 128 - 1), oob_is_err=False),
                ["widx"], [("bwd", b)], dma=True)

        def stA(j):
            b, q = j % NBX, j % 2
            for kc in range(8):
                self.tr(psb[q][:, kc * 128:(kc + 1) * 128], xg[b][:, kc * 128:(kc + 1) * 128], self.ident_b[:], r=[("xg", b), "ident_b"], w=[("ps", q)])
            self.copy("act", xgT[q][:].rearrange("p k t -> p (k t)"), psb[q][:, 0:1024], r=[("ps", q)], w=[("xgT", q)])

        def stB(j):
            b, q = j % NB, j % 2
            for kc in range(8):
                self.mm(ps[2 + q][:, :], xgT[q][:, kc, :], wgu[b][:, kc, :], start=(kc == 0), stop=(kc == 7), r=[("xgT", q), ("bwgu", b)], w=[("ps", 2 + q)])
            self.act(sl[q][:], ps[2 + q][:, 0:256], AF.Silu, r=[("ps", 2 + q)], w=[("bsl", q)])
            self.tt("dve", av[q][:], ps[2 + q][:, 256:512], sl[q][:], ALU.mult, r=[("ps", 2 + q), ("bsl", q)], w=[("bav", q)])

        def stC(j):
            q = j % 2
            for fc in range(2):
                self.tr(psb[4 + q][:, fc * 128:(fc + 1) * 128], av[q][:, fc * 128:(fc + 1) * 128], self.ident_b[:], r=[("bav", q), "ident_b"], w=[("ps", 4 + q)])
            self.copy("dve", aT[q][:].rearrange("p k t -> p (k t)"), psb[4 + q][:, 0:256], r=[("ps", 4 + q)], w=[("baT", q)])

        def stD(j):
            b, q = j % NB, j % 2
            for half in range(2):
                pb = 6 + half
                for fc in range(2):
                    self.mm(ps[pb][:, :], aT[q][:, fc, :], wd[b][:, fc, half * 512:(half + 1) * 512], start=(fc == 0), stop=(fc == 1),
                            r=[("baT", q), ("bwd", b)], w=[("ps", pb)])
                self.copy("act" if half == 0 else "dve", yb[q][:, half * 512:(half + 1) * 512], ps[pb][:, :], r=[("ps", pb)], w=[("yb", q)])
            self.dma(self.ys[j * BLK:(j + 1) * BLK, :], yb[q][:], r=[("yb", q)], w=[("ys", j)], eng="act")

        for j0 in range(NB - 1):
            bload(j0)
        for j0 in range(NBX - 1):
            xload_(j0)
        stA(0)
        for j in range(NBLK):
            if j + 1 < NBLK:
                stA(j + 1)
            stB(j)
            if j >= 1:
                stD(j - 1)
            stC(j)
            bload(j + NB - 1)
            xload_(j + NBX - 1)
        stD(NBLK - 1)
        self.S.fence()
        M.release(self.e_mark1)
        wsg = M.alloc("wsg", [128, 8, 512], BF16)
        wsd = M.alloc("wsd", [128, 2, D], BF16)
        gfin = M.alloc("gfin", [128, D], F32)
        yk = [[M.alloc("yk%d_%d" % (a_, k), [128, D], BF16) for k in range(6)] for a_ in range(3)]
        acc = [M.alloc("acc%d" % i, [128, D], F32) for i in range(3)]
        xo = [M.alloc("xo%d" % i, [128, D], F32) for i in range(3)]
        dgs = [M.alloc("dg%d" % i_, [128, 6, 128], BF16) for i_ in range(3)]
        ssl = M.alloc("ssl", [128, 256], F32)
        sav = M.alloc("sav", [128, 256], BF16)
        saT = M.alloc("saT", [128, 2, 128], BF16)
        assert M.mark() <= self.oT_end, M.mark()
        self.dma(wsg[:, :, 0:256], I["w_sh_gate"].rearrange("(kc p) n -> p kc n", p=128), w=["wsg"], eng="pool")
        self.dma(wsg[:, :, 256:512], I["w_sh_up"].rearrange("(kc p) n -> p kc n", p=128), w=["wsg"], eng="pool")
        self.dma(wsd[:], I["w_sh_down"].rearrange("(fc p) n -> p fc n", p=128), w=["wsd"], eng="pool")
        self.dma(gfin[:], I["g_final"].partition_broadcast(128), w=["gfin"])

        def gload(i):
            if i >= 16:
                return
            a_ = i % 3
            for k in range(6):
                self.S.op("pool", (lambda k, a_, i: lambda e: e.indirect_dma_start(
                    out=yk[a_][k][:], out_offset=None, in_=self.ys, in_offset=bass.IndirectOffsetOnAxis(ap=self.dest6[:, i, k:k + 1], axis=0),
                    bounds_check=self.bc_reg(e, NSLOT - 1), oob_is_err=False))(k, a_, i), [], [("yk", a_, k)], dma=True)
            self.dma(xo[a_][:], self.scr_x2[i * 128:(i + 1) * 128, :], w=[("xo", a_)])

        gload(0)
        gload(1)
        for i in range(16):
            gload(i + 2)
            a_ = i % 3
            H2 = self.h2T[i // 8]
            col0 = (i % 8) * 128
            for kc in range(8):
                self.mm(ps[0][:, :], H2[:, kc, col0:col0 + 128], wsg[:, kc, :], start=(kc == 0), stop=(kc == 7), r=["wsg"], w=[("ps", 0)])
            self.act(ssl[:], ps[0][:, 0:256], AF.Silu, r=[("ps", 0)], w=["ssl"])
            self.tt("dve", sav[:], ps[0][:, 256:512], ssl[:], ALU.mult, r=[("ps", 0), "ssl"], w=["sav"])
            for fc in range(2):
                self.tr(psb[1][:, fc * 128:(fc + 1) * 128], sav[:, fc * 128:(fc + 1) * 128], self.ident_b[:], r=["sav", "ident_b"], w=[("ps", 1)])
            self.copy("dve", saT[:].rearrange("p k t -> p (k t)"), psb[1][:, 0:256], r=[("ps", 1)], w=["saT"])
            A = acc[a_]
            dg = dgs[a_]
            for k in range(6):
                self.ts("dve", dg[:, k, :], self.ident_b[:], self.gate6[:, i, k:k + 1], None, ALU.mult, r=["ident_b"], w=[("dg", a_)])
            for half in range(2):
                pb = 2 + half
                hs = slice(half * 512, (half + 1) * 512)
                for fc in range(2):
                    self.mm(ps[pb][:, :], saT[:, fc, :], wsd[:, fc, hs], start=(fc == 0), stop=False, r=["saT", "wsd"], w=[("ps", pb)])
                for k in range(6):
                    self.mm(ps[pb][:, :], dg[:, k, :], yk[a_][k][:, hs], start=False, stop=(k == 5), r=[("dg", a_), ("yk", a_, k)], w=[("ps", pb)])
                self.tt("dve", A[:, hs], ps[pb][:, :], self.gf_bc[:, hs], ALU.mult, r=[("ps", pb), "gf_bc"], w=[("acc", a_)])
            X = xo[a_]
            self.tt("dve", X[:], X[:], A[:], ALU.add, r=[("acc", a_), ("xo", a_)], w=[("xo", a_)])
            c = self.stat_col()
            ssq, ms, rstd = self.stat[:, 0, c:c + 1], self.stat[:, 1, c:c + 1], self.stat[:, 2, c:c + 1]
            sk = ("st", c)
            self.stt("dve", A[:], X[:], 1.0, X[:], ALU.mult, ALU.mult, r=[("xo", a_)], w=[(sk, 0), ("acc", a_)], accum_out=ssq)
            self.ts("dve", ms, ssq, 1.0 / D, EPS, ALU.mult, ALU.add, r=[(sk, 0)], w=[(sk, 1)])
            self.tt("pool", rstd, ms, self.mhalf[:], ALU.pow, r=[(sk, 1), "mhalf"], w=[(sk, 2)])
            self.stt("dve", X[:], X[:], rstd, gfin[:], ALU.mult, ALU.mult, r=[("xo", a_), (sk, 2), "gfin"], w=[("xo", a_)])
            self.dma(self.out[i * 128:(i + 1) * 128, :], X[:], r=[("xo", a_)], w=[("out", i)])

    def build(self):
        self.declare_inputs()
        self.precast = self.moe_precast_gen() if self.stage >= 5 else None
        self.setup_persistent()
        self.phase_a()
        self.phase_b()
        if self.stage >= 2:
            self.pass_mla()
        if self.stage >= 3:
            self.pass_fox()
        if self.stage >= 4:
            self.phase_d()
        if self.stage >= 5:
            self.phase_e_sparse()
        else:
            pass
        nc = self.nc
        sems = {e: nc.alloc_semaphore("s_" + e) for e in ENGS}
        dsems = [nc.alloc_semaphore("d%d" % i) for i in range(N_DSEM)]
        self.S.emit(sems, dsems)
        return nc


def host_inputs(inputs, core):
    b, p = core // 2, core % 2
    f32 = np.float32
    x = np.asarray(inputs["x"])[b]
    pos = np.asarray(inputs["positions"])[b].astype(np.int32)
    own = (np.arange(16)[:, None] * 2 + p) * 128 + np.arange(128)[None, :]
    own = own.reshape(-1)
    c = np.asarray(inputs["c"])[b]
    bmod = np.asarray(inputs["b_mod"])[0]

    def fm(v, n):
        return np.ascontiguousarray(np.asarray(v).reshape(n, 128).T).astype(f32)

    bmod_fm = np.concatenate([fm(bmod[s * 1024:(s + 1) * 1024], 8) for s in (0, 1, 3, 4)], axis=1)
    bmod_g = np.stack([bmod[2048:3072], bmod[5120:6144]]).astype(f32)
    bfg = np.asarray(inputs["b_forget"])[0]
    bf72 = np.zeros((128, 1), f32)
    for o in (0, 32, 64):
        bf72[o:o + 8, 0] = bfg
    invf = np.zeros((128, 1), f32)
    for r in range(128):
        invf[r, 0] = 10000.0 ** (-((r % 32) % 16) / 16.0)
    pvec = np.zeros((128, 2), f32)
    pvec[:, 0] = p
    pvec[:, 1] = 1 - p
    tri = (np.arange(128)[:, None] <= np.arange(128)[None, :]).astype(f32)
    mask0 = tri if p == 0 else np.ones((128, 128), f32)
    mask1 = np.zeros((128, 128), f32) if p == 0 else tri
    esel = np.zeros((128, 8, 128), f32)
    for h in range(8):
        for o in (0, 32, 64):
            esel[o + h, h, :] = 8.0
    sel3 = np.zeros((128, 8, 3), f32)
    for h in range(8):
        for m in range(3):
            sel3[32 * m + h, h, m] = 1.0
    d = {
        "x_all": np.ascontiguousarray(x), "x_own": np.ascontiguousarray(x[own]),
        "pos_all": np.ascontiguousarray(pos), "pos_own": np.ascontiguousarray(pos[own]),
        "c_t": fm(c, 8),
        "w_mod": np.asarray(inputs["w_mod"])[0], "bmod_fm": np.ascontiguousarray(bmod_fm), "bmod_g": bmod_g,
        "g_mix_fm": fm(np.asarray(inputs["g_mix_norm"])[0], 8), "g_ffn_fm": fm(np.asarray(inputs["g_ffn_norm"])[0], 8),
        "g_final": np.asarray(inputs["g_final"]).astype(f32),
        "w_in": np.asarray(inputs["w_in"])[0], "bf72": bf72,
        "gq_fm": fm(np.asarray(inputs["g_q_lat"])[0], 2), "gkv_fm": fm(np.asarray(inputs["g_kv_lat"])[0], 1),
        "w_q_up": np.asarray(inputs["w_q_up"])[0], "w_kv_up": np.asarray(inputs["w_kv_up"])[0],
        "w_o_mla": np.asarray(inputs["w_o_mla"])[0], "w_o_fox": np.asarray(inputs["w_o_fox"])[0], "w_out": np.asarray(inputs["w_out"])[0],
        "w_router": np.asarray(inputs["w_router"])[0], "b_router": np.asarray(inputs["b_router"])[0],
        "w_exp_gate": np.asarray(inputs["w_exp_gate"])[0], "w_exp_up": np.asarray(inputs["w_exp_up"])[0],
        "w_exp_down": np.asarray(inputs["w_exp_down"])[0],
        "w_sh_gate": np.asarray(inputs["w_sh_gate"])[0], "w_sh_up": np.asarray(inputs["w_sh_up"])[0], "w_sh_down": np.asarray(inputs["w_sh_down"])[0],
        "ident": np.eye(128, dtype=f32), "invf": invf, "pvec": pvec, "mask0": np.ascontiguousarray(mask0), "mask1": np.ascontiguousarray(mask1),
        "esel": esel.reshape(128, 1024),
        "sel3": sel3.reshape(128, 24),
        "thr16": np.broadcast_to((np.arange(16) * 128.0).astype(f32), (128, 16)),
        "thrj": np.broadcast_to((np.arange(NBLK) * float(BLK)).astype(f32), (128, NBLK)),
        "iota_p": np.arange(128, dtype=f32).reshape(128, 1),
        "triu": (np.arange(128)[:, None] < np.arange(128)[None, :]).astype(f32),
    }
    return {k: np.ascontiguousarray(v) for k, v in d.items()}, own


def kernel(**inputs):
    n = 8
    prog = Prog(stage=5, debug=False)
    nc = prog.build()
    maps, owns = [], []
    for core in range(n):
        m, own = host_inputs(inputs, core)
        maps.append(m)
        owns.append(own)
    res = run_bass_kernel_spmd(nc, maps, core_ids=list(range(n)))
    out = np.zeros((4, SEQ, D), np.float32)
    for core in range(n):
        out[core // 2, owns[core], :] = np.asarray(res.results[core]["out"], dtype=np.float32)
    return out
```
